# Optimizing a Trainium2 kernel written in Bass

```python
import jax, jax.numpy as jnp
from jax import lax
import numpy as np

D_MODEL = 1024
BATCH = 8
SEQ = 2048
DEPTH = 4

GLA_HEADS = 4
GLA_DK = 64
GLA_DV = 128
GLA_KEY = GLA_HEADS * GLA_DK
GLA_VAL = GLA_HEADS * GLA_DV
GLA_RANK = 16
GLA_TAU = 16.0
GLA_CHUNK = 64
SGU_HEADS = 4
SGU_CHUNK = 128
SGU_W = 512
SGU_DH = SGU_W // SGU_HEADS
CONV_GROUPS = 4
CONV_W = 512
CONV_K = 3
N_BRANCH = 3
D_FF = 2816
FFN_CONV_K = 3
EPS = 1e-6

IN_SPLITS = (GLA_KEY, GLA_KEY, GLA_VAL, GLA_VAL, GLA_RANK, SGU_W, SGU_W, CONV_W, CONV_W, CONV_W, N_BRANCH * D_MODEL)
IN_COLS = 2 * GLA_KEY + 2 * GLA_VAL + GLA_RANK + 2 * SGU_W + 3 * CONV_W + N_BRANCH * D_MODEL

kernel_name = 'hybrid_gla_sgu_shortconv_convffn'


def rms_norm(x, g):
    xf = x.astype(jnp.float32)
    y = xf * lax.rsqrt(jnp.mean(xf * xf, axis=-1, keepdims=True) + EPS)
    return (y * g.astype(jnp.float32)).astype(x.dtype)


def layer_norm(x, g, b):
    xf = x.astype(jnp.float32)
    mu = jnp.mean(xf, axis=-1, keepdims=True)
    xc = xf - mu
    y = xc * lax.rsqrt(jnp.mean(xc * xc, axis=-1, keepdims=True) + EPS)
    return (y * g.astype(jnp.float32) + b.astype(jnp.float32)).astype(x.dtype)


def causal_dwconv(x, w):
    K = w.shape[0]
    S = x.shape[1]
    xp = jnp.pad(x, ((0, 0), (K - 1, 0), (0, 0)))
    y = xp[:, 0:S] * w[0]
    for kk in range(1, K):
        y = y + xp[:, kk:kk + S] * w[kk]
    return y


def gla_chunked(q, k, v, log_a):
    Bsz, S, H, dk = q.shape
    dv = v.shape[-1]
    C = GLA_CHUNK
    n = S // C
    f32 = jnp.float32
    q, k, v, log_a = (t.astype(f32).reshape(Bsz, n, C, H, t.shape[-1]) for t in (q, k, v, log_a))
    b = jnp.cumsum(log_a, axis=2)
    b_last = b[:, :, -1:]
    q_t = q * jnp.exp(b)
    k_t = k * jnp.exp(-b)
    k_end = k * jnp.exp(b_last - b)
    causal = jnp.tril(jnp.ones((C, C), dtype=bool))
    scores = jnp.einsum('bnihk,bnjhk->bnhij', q_t, k_t)
    scores = jnp.where(causal, scores, 0.0)
    o_intra = jnp.einsum('bnhij,bnjhv->bnihv', scores, v)
    chunk_state = jnp.einsum('bnjhk,bnjhv->bnhkv', k_end, v)
    decay = jnp.exp(b_last[:, :, 0])

    def step(s_prev, inp):
        dec, cs = inp
        return dec[..., None] * s_prev + cs, s_prev

    s0 = jnp.zeros((Bsz, H, dk, dv), f32)
    _, s_prevs = lax.scan(step, s0, (jnp.moveaxis(decay, 1, 0), jnp.moveaxis(chunk_state, 1, 0)))
    s_prevs = jnp.moveaxis(s_prevs, 0, 1)
    o_inter = jnp.einsum('bnihk,bnhkv->bnihv', q_t, s_prevs)
    return (o_intra + o_inter).reshape(Bsz, S, H, dv)


def sgu_chunked(u, v, ln_g, ln_b, ws, bs):
    Bsz, S, _ = v.shape
    n = S // SGU_CHUNK
    v = layer_norm(v, ln_g, ln_b)
    v = v.reshape(Bsz, n, SGU_CHUNK, SGU_HEADS, SGU_DH)
    ws_causal = jnp.where(jnp.tril(jnp.ones((SGU_CHUNK, SGU_CHUNK), dtype=bool)), ws, 0.0)
    mixed = jnp.einsum('hts,bnshd->bnthd', ws_causal, v) + jnp.transpose(bs)[None, None, :, :, None]
    return u * mixed.reshape(Bsz, S, SGU_W)


def setup_inputs(seed: int = 0) -> dict:
    key = jax.random.key(seed)
    ks = jax.random.split(key, 24)
    f32 = jnp.float32

    def nrm(k, shape, scale):
        return jax.random.normal(k, shape, f32) * scale

    L = DEPTH
    return {
        'x': nrm(ks[0], (BATCH, SEQ, D_MODEL), 1.0),
        'norm_mix': 1.0 + nrm(ks[1], (L, D_MODEL), 0.02),
        'w_in': nrm(ks[2], (L, D_MODEL, IN_COLS), D_MODEL ** -0.5),
        'gla_w_alpha': nrm(ks[3], (L, GLA_RANK, GLA_KEY), GLA_RANK ** -0.5),
        'gla_b_alpha': nrm(ks[4], (L, GLA_KEY), 0.1),
        'gla_norm': 1.0 + nrm(ks[5], (L, GLA_VAL), 0.02),
        'gla_w_out': nrm(ks[6], (L, GLA_VAL, D_MODEL), GLA_VAL ** -0.5),
        'sgu_ln_g': 1.0 + nrm(ks[7], (L, SGU_W), 0.02),
        'sgu_ln_b': nrm(ks[8], (L, SGU_W), 0.02),
        'sgu_ws': nrm(ks[9], (L, SGU_HEADS, SGU_CHUNK, SGU_CHUNK), SGU_CHUNK ** -0.5),
        'sgu_bs': 1.0 + nrm(ks[10], (L, SGU_HEADS, SGU_CHUNK), 0.02),
        'sgu_w_out': nrm(ks[11], (L, SGU_W, D_MODEL), SGU_W ** -0.5),
        'conv_w': nrm(ks[12], (L, CONV_K, CONV_W), CONV_K ** -0.5),
        'conv_w_out': nrm(ks[13], (L, CONV_W, D_MODEL), CONV_W ** -0.5),
        'w_o': nrm(ks[14], (L, D_MODEL, D_MODEL), D_MODEL ** -0.5),
        'norm_ffn': 1.0 + nrm(ks[15], (L, D_MODEL), 0.02),
        'ffn_w_up': nrm(ks[16], (L, D_MODEL, 2 * D_FF), D_MODEL ** -0.5),
        'ffn_conv_w': nrm(ks[17], (L, FFN_CONV_K, 2 * D_FF), FFN_CONV_K ** -0.5),
        'ffn_conv_b': nrm(ks[18], (L, 2 * D_FF), 0.01),
        'ffn_w_down': nrm(ks[19], (L, D_FF, D_MODEL), D_FF ** -0.5),
        'norm_final': 1.0 + nrm(ks[20], (D_MODEL,), 0.02),
    }


def reference(x, norm_mix, w_in, gla_w_alpha, gla_b_alpha, gla_norm, gla_w_out,
              sgu_ln_g, sgu_ln_b, sgu_ws, sgu_bs, sgu_w_out, conv_w, conv_w_out,
              w_o, norm_ffn, ffn_w_up, ffn_conv_w, ffn_conv_b, ffn_w_down, norm_final):
    Bsz, S, D = x.shape
    split_points = np.cumsum(IN_SPLITS)[:-1].tolist()
    for l in range(DEPTH):
        h = rms_norm(x, norm_mix[l])
        proj = h @ w_in[l]
        (q, k, v, g_out, a_lr, su, sv, cb, cc, cx, gate_logits) = jnp.split(proj, split_points, axis=-1)

        log_a = jax.nn.log_sigmoid((a_lr @ gla_w_alpha[l] + gla_b_alpha[l]).astype(jnp.float32)) / GLA_TAU
        o = gla_chunked(q.reshape(Bsz, S, GLA_HEADS, GLA_DK) * (GLA_DK ** -0.5),
                        k.reshape(Bsz, S, GLA_HEADS, GLA_DK),
                        v.reshape(Bsz, S, GLA_HEADS, GLA_DV),
                        log_a.reshape(Bsz, S, GLA_HEADS, GLA_DK))
        o = rms_norm(o, gla_norm[l].reshape(GLA_HEADS, GLA_DV)).astype(x.dtype).reshape(Bsz, S, GLA_VAL)
        y_a = (o * jax.nn.silu(g_out)) @ gla_w_out[l]

        y_b = sgu_chunked(jax.nn.gelu(su), jax.nn.gelu(sv), sgu_ln_g[l], sgu_ln_b[l],
                          sgu_ws[l], sgu_bs[l]) @ sgu_w_out[l]

        y_c = (cb * causal_dwconv(cc * cx, conv_w[l])) @ conv_w_out[l]

        gt = jax.nn.sigmoid(gate_logits).reshape(Bsz, S, N_BRANCH, D)
        merged = gt[:, :, 0] * y_a + gt[:, :, 1] * y_b + gt[:, :, 2] * y_c
        x = x + merged @ w_o[l]

        h = rms_norm(x, norm_ffn[l])
        a = causal_dwconv(h @ ffn_w_up[l], ffn_conv_w[l]) + ffn_conv_b[l]
        gate, val = jnp.split(a, 2, axis=-1)
        x = x + (jax.nn.silu(gate) * val) @ ffn_w_down[l]
    return rms_norm(x, norm_final)
```

```python
from contextlib import ExitStack
import numpy as np
import concourse.bass as bass
import concourse.mybir as mybir
from concourse.bass_utils import run_bass_kernel_spmd

F32 = mybir.dt.float32
BF16 = mybir.dt.bfloat16
AF = mybir.ActivationFunctionType
ALU = mybir.AluOpType

D = 1024
NK = 8
INC = 7184
DFF = 2816
NF = 22
EPS = 1e-6
CV_L = 208


class _Op:
    __slots__ = ("eng", "fn", "deps", "dma", "ndma", "signal", "count", "sem")


class Sched:
    ENGS = ("pe", "act", "dve", "pool", "sp")

    def __init__(self):
        self.ops = []
        self.by_eng = {e: [] for e in self.ENGS}
        self.last_w = {}
        self.readers = {}

    def add(self, eng, fn, reads=(), writes=(), dma=None, ndma=1):
        op = _Op()
        op.eng, op.fn, op.dma, op.ndma = eng, fn, dma, ndma
        op.signal = dma is not None
        op.count = 0
        op.sem = None
        deps = {}

        def need(d, raw):
            if d.dma is not None or dma is not None:
                return True
            if d.eng != eng:
                return True
            if eng == "pe":
                return False
            return True

        for r in reads:
            w = self.last_w.get(r)
            if w is not None and need(w, True):
                deps[id(w)] = w
        for r in writes:
            w = self.last_w.get(r)
            if w is not None and need(w, False):
                deps[id(w)] = w
            for rd in self.readers.get(r, {}).values():
                if rd is not op and need(rd, False):
                    deps[id(rd)] = rd
        op.deps = list(deps.values())
        for d in op.deps:
            d.signal = True
        for r in reads:
            self.readers.setdefault(r, {})[eng if dma is None else ("dma", len(self.ops))] = op
        for r in writes:
            self.last_w[r] = op
            self.readers[r] = {}
        self.ops.append(op)
        self.by_eng[eng].append(op)
        return op

    def emit(self, nc, es, final_waits=()):
        sems = {}

        def get_sem(key):
            if key not in sems:
                sems[key] = es.enter_context(nc.semaphore("s_%s_%s" % key))
            return sems[key]

        cnt = {}
        for op in self.ops:
            if op.dma is not None:
                key = ("d", op.dma)
                cnt[key] = cnt.get(key, 0) + 16 * op.ndma
                op.sem, op.count = key, cnt[key]
            elif op.signal:
                key = ("e", op.eng)
                cnt[key] = cnt.get(key, 0) + 1
                op.sem, op.count = key, cnt[key]
        for k in cnt:
            get_sem(k)
        block = es.enter_context(nc.Block())
        sections = {"pe": block.tensor, "act": block.scalar, "dve": block.vector,
                    "pool": block.gpsimd, "sp": block.sync}

        def make(engname):
            oplist = self.by_eng[engname]

            def body(e):
                waited = {}
                for op in oplist:
                    need = {}
                    for d in op.deps:
                        if need.get(d.sem, 0) < d.count:
                            need[d.sem] = d.count
                    for k, v in need.items():
                        if waited.get(k, 0) < v:
                            e.wait_ge(sems[k], v)
                            waited[k] = v
                    if op.dma is not None:
                        op.fn(e, sems[op.sem])
                    else:
                        ins = op.fn(e)
                        if op.signal:
                            ins.then_inc(sems[op.sem], 1)
                if engname == "sp":
                    for op in final_waits:
                        e.wait_ge(sems[op.sem], op.count)
            return body

        for en in self.ENGS:
            sections[en](make(en))


STAGE = 99
SUB = 99


def build(T=2048, NL=4):
    NST = T // 512
    GST = 256
    nc = bass.Bass("TRN2", target_bir_lowering=False)
    es = ExitStack()

    def dram(name, shape, kind="ExternalInput"):
        return nc.dram_tensor(name, list(shape), F32, kind=kind).ap()

    xT_d = dram("xT", [D, T])
    w_in_d = dram("w_in", [NL, D, INC])
    wA_d = [dram(n, [NL, 512, D]) for n in ("gla_w_out", "sgu_w_out", "conv_w_out")]
    w_o_d = dram("w_o", [NL, D, D])
    w_up_d = dram("ffn_w_up", [NL, D, 2 * DFF])
    w_dn_d = dram("ffn_w_down", [NL, DFF, D])
    walpha_d = dram("walpha17", [NL, 17, 256])
    wsT_d = dram("wsT", [NL, 128, 512])
    cvec_d = dram("cvec", [128, NL * CV_L + 8])
    rowb_d = dram("rowb", [NL, 128, 1536])
    cmat_d = dram("cmat", [128, 768])
    yT_d = dram("yT", [D, T], kind="ExternalOutput")

    def sb(name, shape, dt):
        return es.enter_context(nc.sbuf_tensor(name, list(shape), dt))

    xT = sb("xT_s", [128, NK, T], F32)
    hT = sb("hT_s", [128, NK, T], BF16)
    BR = sb("BR_s", [128, 4, T], BF16)
    WO = sb("WO_s", [128, NK, D], BF16)
    SLOT_E = 8320
    SLOT = [sb("slot%d" % i, [128, SLOT_E], BF16) for i in range(2)]
    SLW = sb("slotW", [128, 4, D], BF16)
    NFB, NHB = 6, 16
    Fb = [sb("F%d" % i, [128, 516], F32) for i in range(NFB)]
    Hb = [sb("H%d" % i, [128, 512], BF16) for i in range(NHB)]
    cvf = sb("cvf", [128, 8], F32)
    cvl = sb("cvl", [128, CV_L], F32)
    triU = sb("triU", [128, 128], BF16)
    triSU = sb("triSU", [128, 128], BF16)
    MASK = sb("MASK", [128, 512], BF16)
    ones_bf = sb("ones_bf", [128, 128], BF16)
    epsc = sb("epsc", [128, 1], F32)
    A17 = sb("A17", [32, GST], BF16)
    walpha = sb("walpha", [32, 256], BF16)
    WST = sb("WST", [128, 512], BF16)
    LNG = sb("LNG", [128, 512], BF16)
    LNB = sb("LNB", [128, 512], BF16)
    BSB = sb("BSB", [128, 512], F32)
    Sst = sb("Sst", [128, 256], F32)
    Sbf = sb("Sbf", [128, 256], BF16)
    DEC = sb("DEC", [128, 2], F32)
    STATS = [sb("STAT%d" % i, [128, 8], F32) for i in range(2)]
    MVS = [sb("MV%d" % i, [128, 4], F32) for i in range(2)]
    PS = [es.enter_context(nc.psum_tensor("ps%d" % i, [128, 512], F32)) for i in range(8)]

    S = Sched()
    bank_ctr = [0]

    def nb():
        b = bank_ctr[0] % 8
        bank_ctr[0] += 1
        return b

    def mm(bank, out_ap, pairs, reads):
        def fn(pe):
            n = len(pairs)
            ins = None
            for i, (l, r) in enumerate(pairs):
                ins = pe.matmul(out_ap, l, r, start=(i == 0), stop=(i == n - 1))
            return ins
        return S.add("pe", fn, reads=reads, writes=[("ps", bank)])

    def act(out, in_, func, reads, writes, bias=None, scale=None):
        kw = {}
        if bias is not None:
            kw["bias"] = bias
        if scale is not None:
            kw["scale"] = scale
        return S.add("act", lambda e: e.activation(out=out, in_=in_, func=func, **kw), reads=reads, writes=writes)

    def tt(out, in0, in1, op, reads, writes, eng="dve"):
        return S.add(eng, lambda e: e.tensor_tensor(out=out, in0=in0, in1=in1, op=op), reads=reads, writes=writes)

    def ts(out, in0, s1, s2, op0, op1, reads, writes, eng="dve"):
        if s2 is None:
            return S.add(eng, lambda e: e.tensor_scalar(out=out, in0=in0, scalar1=s1, scalar2=None, op0=op0),
                         reads=reads, writes=writes)
        return S.add(eng, lambda e: e.tensor_scalar(out=out, in0=in0, scalar1=s1, scalar2=s2, op0=op0, op1=op1),
                     reads=reads, writes=writes)

    def stt(out, in0, scalar, in1, op0, op1, reads, writes, eng="dve"):
        return S.add(eng, lambda e: e.scalar_tensor_tensor(out=out, in0=in0, scalar=scalar, in1=in1, op0=op0, op1=op1),
                     reads=reads, writes=writes)

    def cp(eng, out, in_, reads, writes):
        if eng == "act":
            return S.add(eng, lambda e: e.activation(out=out, in_=in_, func=AF.Copy), reads=reads, writes=writes)
        return S.add(eng, lambda e: e.tensor_copy(out=out, in_=in_), reads=reads, writes=writes)

    def ms(eng, ap, val, writes):
        return S.add(eng, lambda e: e.memset(ap, val), reads=[], writes=writes)

    def dma(eng, pairs, writes, semkey, reads=()):
        def fn(e, sem):
            for o, i in pairs:
                e.dma_start(out=o, in_=i).then_inc(sem, 16)
        return S.add(eng, fn, reads=reads, writes=writes, dma=semkey, ndma=len(pairs))

    def v3(ap, a):
        return ap.rearrange("p (a b) -> p a b", a=a)

    dma("sp", [(xT[:, k, :], xT_d[k * 128:(k + 1) * 128, :]) for k in range(NK)],
        [("xT", k, st) for k in range(NK) for st in range(NST)], "init")
    dma("sp", [(cvf[:, :], cvec_d[:, NL * CV_L:NL * CV_L + 8])], ["cvf"], "init2")
    dma("pool", [(MASK[:, :], cmat_d[:, 256:768]), (triU[:, :], cmat_d[:, 0:128]), (triSU[:, :], cmat_d[:, 128:256])],
        ["MASK", "tri"], "initp")
    ms("dve", ones_bf[:, :], 1.0, ["ones"])
    ms("dve", epsc[:, :], EPS, ["eps"])
    ms("dve", A17[:, :], 1.0, ["A17"])

    def rmsnorm_rstd(st, tsl):
        b = nb()
        sqs = []
        for k in range(NK):
            hb = Hb[8 + (k % 2)]
            hres = ("H", 8 + (k % 2))
            act(hb[:, :], xT[:, k, tsl], AF.Square, [("xT", k, st)], [hres])
            sqs.append((hb, hres))
            def fn(pe, k=k, hb=hb, b=b):
                return pe.matmul(PS[b][:, :], ones_bf[:, :], hb[:, :], start=(k == 0), stop=(k == NK - 1))
            S.add("pe", fn, reads=[hres, "ones"], writes=[("ps", b)])
        act(Fb[0][:, 0:512], PS[b][:, :], AF.Ln, [("ps", b), "eps"], [("F", 0)], bias=epsc[:, 0:1], scale=1.0 / D)
        act(Fb[0][:, 0:512], Fb[0][:, 0:512], AF.Exp, [("F", 0)], [("F", 0)], scale=-0.5)
        return 0

    def norm_to_h(l_col):
        for st in range(NST):
            tsl = slice(st * 512, (st + 1) * 512)
            rmsnorm_rstd(st, tsl)
            for k in range(NK):
                stt(hT[:, k, tsl], xT[:, k, tsl], cvl[:, l_col + k:l_col + k + 1], Fb[0][:, 0:512],
                    ALU.mult, ALU.mult, [("xT", k, st), ("F", 0), "cvl"], [("hT", st)])

    slot_ctr = [0]

    def next_slot():
        s = slot_ctr[0] % 2
        slot_ctr[0] += 1
        return s

    def load_group(s, pairs):
        dma("pool", pairs, [("slot", s)], "slot%d" % s)

    for l in range(NL):
        dma("sp", [(cvl[:, :], cvec_d[:, l * CV_L:(l + 1) * CV_L]),
                   (BSB[:, :], rowb_d[l, :, 1024:1536])], ["cvl", "BSB"], "lsmall")
        dma("pool", [(WST[:, :], wsT_d[l]), (LNG[:, :], rowb_d[l, :, 0:512]), (LNB[:, :], rowb_d[l, :, 512:1024]),
                     (walpha[0:17, :], walpha_d[l])],
            ["WST", "LNG", "LNB", "walpha"], "lsmallp")
        tt(WST[:, :], WST[:, :], MASK[:, :], ALU.mult, ["WST", "MASK"], ["WST"])
        dma("pool", [(WO[:, k, :], w_o_d[l, k * 128:(k + 1) * 128, :]) for k in range(NK)], ["WO"], "wo")

        if STAGE < 1:
            continue
        norm_to_h(0)
        if STAGE < 2:
            continue

        s = next_slot()
        Wg = SLOT[s][:, 0:NK * 512].rearrange("p (k c) -> p k c", k=NK)
        load_group(s, [(Wg[:, k, :], w_in_d[l, k * 128:(k + 1) * 128, 1024:1536]) for k in range(NK)])
        i = 0
        for st in range(NST):
            tsl = slice(st * 512, (st + 1) * 512)
            for h in range(4):
                b = nb()
                mm(b, PS[b][:, :], [(Wg[:, k, h * 128:(h + 1) * 128], hT[:, k, tsl]) for k in range(NK)],
                   [("slot", s), ("hT", st)])
                hb = 8 + (i % 2)
                i += 1
                act(Hb[hb][:, :], PS[b][:, :], AF.Sigmoid, [("ps", b)], [("H", hb)])
                stt(BR[:, h, tsl], PS[b][:, :], cvl[:, 16 + h:17 + h], Hb[hb][:, :], ALU.mult, ALU.mult,
                    [("ps", b), ("H", hb), "cvl"], [("BR", st)])

        if STAGE < 3:
            continue
        s = next_slot()
        Wm = SLOT[s][:, 0:NK * 1040].rearrange("p (k c) -> p k c", k=NK)
        prs = []
        for k in range(NK):
            prs.append((Wm[:, k, 0:1024], w_in_d[l, k * 128:(k + 1) * 128, 0:1024]))
            prs.append((Wm[:, k, 1024:1040], w_in_d[l, k * 128:(k + 1) * 128, 1536:1552]))
        load_group(s, prs)
        slr = ("slot", s)
        ms("dve", Sst[:, :], 0.0, ["S"])
        ms("dve", Sbf[:, :], 0.0, ["Sbf"])
        EB, ENB, QT, KT, Vb, KEb, STM, OSQ, TT_ = 0, 1, 2, 3, 4, 5, 6, 7, 8
        QTO = 10
        ms("dve", Hb[QT][:, :], 0.0, [("H", QT)])
        ms("dve", Hb[QTO][:, :], 0.0, [("H", QTO)])
        FELA, FR = 1, 2
        for gst in range(T // GST):
            g0 = gst * GST
            gsl = slice(g0, g0 + GST)
            st = g0 // 512
            hres = ("hT", st)
            b = nb()
            mm(b, PS[b][0:16, 0:GST], [(Wm[:, k, 1024:1040], hT[:, k, gsl]) for k in range(NK)], [slr, hres])
            if SUB < 1:
                continue
            act(A17[0:16, :], PS[b][0:16, 0:GST], AF.Copy, [("ps", b)], ["A17"])
            if SUB < 2:
                continue
            for t2 in range(GST // 128):
                loc = slice(t2 * 128, (t2 + 1) * 128)
                b = nb()
                mm(b, PS[b][:, 0:256], [(A17[0:17, loc], walpha[0:17, :])], ["A17", "walpha"])
                act(Fb[FELA][:, 0:256], PS[b][:, 0:256], AF.Exp, [("ps", b)], [("F", FELA)], scale=-1.0)
                act(Fb[FELA][:, 256:512], Fb[FELA][:, 0:256], AF.Ln, [("F", FELA)], [("F", FELA)], bias=1.0, scale=1.0)
                la = Fb[FELA]
                LAH, LAL = Hb[9][:, 0:256], Hb[9][:, 256:512]
                cp("dve", LAH, la[:, 256:512], [("F", FELA)], [("H", 9)])
                tt(LAL, la[:, 256:512], LAH, ALU.subtract, [("F", FELA), ("H", 9)], [("H", 9)])
                if SUB < 3:
                    continue
                b = nb()
                for cc in range(2):
                    mm(b, PS[b][:, cc * 128:(cc + 1) * 128],
                       [(LAH[:, cc * 128:(cc + 1) * 128], triU[:, :]), (LAL[:, cc * 128:(cc + 1) * 128], triU[:, :])],
                       [("H", 9), "tri"])
                mm(b, PS[b][:, 256:512], [(triSU[:, :], LAH), (triSU[:, :], LAL)], [("H", 9), "tri"])
                for cc in range(2):
                    act(Hb[EB][:, cc * GST + t2 * 128:cc * GST + (t2 + 1) * 128], PS[b][:, cc * 128:(cc + 1) * 128],
                        AF.Exp, [("ps", b)], [("H", EB)])
                    act(Hb[ENB][:, cc * GST + t2 * 128:cc * GST + (t2 + 1) * 128], PS[b][:, cc * 128:(cc + 1) * 128],
                        AF.Exp, [("ps", b)], [("H", ENB)], scale=-1.0)
                if SUB < 4:
                    continue
                act(Fb[3 + t2][:, 0:256], PS[b][:, 256:512], AF.Exp, [("ps", b)], [("F", 3 + t2)])
                act(Fb[3 + t2][:, 256:258], PS[b][:, 127:256:128], AF.Exp, [("ps", b)], [("F", 3 + t2)])
            if SUB < 5:
                continue
            for cc in range(2):
                b = nb()
                mm(b, PS[b][:, 0:GST], [(Wm[:, k, cc * 128:(cc + 1) * 128], hT[:, k, gsl]) for k in range(NK)], [slr, hres])
                stt(Hb[QT][0:64, cc * GST:(cc + 1) * GST], PS[b][0:64, 0:GST], 0.125, Hb[EB][0:64, cc * GST:(cc + 1) * GST],
                    ALU.mult, ALU.mult, [("ps", b), ("H", EB)], [("H", QT)])
                stt(Hb[QTO][64:128, cc * GST:(cc + 1) * GST], PS[b][64:128, 0:GST], 0.125, Hb[EB][64:128, cc * GST:(cc + 1) * GST],
                    ALU.mult, ALU.mult, [("ps", b), ("H", EB)], [("H", QTO)])
                b = nb()
                mm(b, PS[b][:, 0:GST], [(Wm[:, k, 256 + cc * 128:256 + (cc + 1) * 128], hT[:, k, gsl]) for k in range(NK)],
                   [slr, hres])
                tt(Hb[KT][:, cc * GST:(cc + 1) * GST], PS[b][:, 0:GST], Hb[ENB][:, cc * GST:(cc + 1) * GST], ALU.mult,
                   [("ps", b), ("H", ENB)], [("H", KT)])
            for t2 in range(GST // 128):
                t0 = g0 + t2 * 128
                tok = slice(t0, t0 + 128)
                fe = 3 + t2
                Vb, KEb, STM, OSQ, TT_ = ((4, 5, 6, 7, 8), (11, 12, 13, 14, 15))[t2]
                FR = (2, 5)[t2]
                if SUB < 6:
                    continue
                b = nb()
                mm(b, PS[b][:, :], [(hT[:, k, tok], Wm[:, k, 512:1024]) for k in range(NK)], [slr, hres])
                act(Hb[Vb][:, :], PS[b][:, :], AF.Copy, [("ps", b)], [("H", Vb)])
                b = nb()
                mm(b, PS[b][:, 0:256], [(hT[:, k, tok], Wm[:, k, 256:512]) for k in range(NK)], [slr, hres])
                tt(Hb[KEb][:, 0:256], PS[b][:, 0:256], Fb[fe][:, 0:256], ALU.mult, [("ps", b), ("F", fe)], [("H", KEb)])
                b = nb()
                for h in range(4):
                    cc, po = h // 2, (h % 2) * 64
                    c0 = cc * GST + t2 * 128
                    qz = QT if h % 2 == 0 else QTO
                    mm(b, PS[b][:, h * 128:(h + 1) * 128],
                       [(Hb[KT][:, c0:c0 + 128], Hb[qz][:, c0:c0 + 128])], [("H", KT), ("H", qz)])
                tt(Hb[STM][:, :], PS[b][:, :], MASK[:, :], ALU.mult, [("ps", b), "MASK"], [("H", STM)])
                if SUB < 7:
                    continue
                bo = nb()
                for h in range(4):
                    cc, po = h // 2, (h % 2) * 64
                    c0 = cc * GST + t2 * 128
                    qz = QT if h % 2 == 0 else QTO
                    mm(bo, PS[bo][:, h * 128:(h + 1) * 128],
                       [(Sbf[:, cc * 128:(cc + 1) * 128], Hb[qz][:, c0:c0 + 128]),
                        (Hb[Vb][:, h * 128:(h + 1) * 128], Hb[STM][:, h * 128:(h + 1) * 128])],
                       ["Sbf", ("H", qz), ("H", Vb), ("H", STM)])
                if SUB < 8:
                    continue
                bc = nb()
                for cc in range(2):
                    mm(bc, PS[bc][:, cc * 256:(cc + 1) * 256],
                       [(Hb[KEb][:, cc * 128:(cc + 1) * 128], Hb[Vb][:, cc * 256:(cc + 1) * 256])], [("H", KEb), ("H", Vb)])
                for h in range(4):
                    cc, po = h // 2, (h % 2) * 64
                    hf = h % 2
                    stt(Sst[po:po + 64, cc * 128:(cc + 1) * 128], Sst[po:po + 64, cc * 128:(cc + 1) * 128],
                        Fb[fe][po:po + 64, 256 + cc:257 + cc],
                        PS[bc][po:po + 64, cc * 256 + hf * 128:cc * 256 + (hf + 1) * 128],
                        ALU.mult, ALU.add, ["S", ("F", fe), ("ps", bc)], ["S"])
                if SUB < 9:
                    continue
                act(Sbf[:, :], Sst[:, :], AF.Copy, ["S"], ["Sbf"])
                act(Hb[OSQ][:, :], PS[bo][:, :], AF.Square, [("ps", bo)], [("H", OSQ)])
                bs_ = nb()
                mm(bs_, PS[bs_][:, :], [(ones_bf[:, :], Hb[OSQ][:, :])], ["ones", ("H", OSQ)])
                act(Fb[FR][:, 0:512], PS[bs_][:, :], AF.Ln, [("ps", bs_), "eps"], [("F", FR)], bias=epsc[:, 0:1], scale=1.0 / 128)
                act(Fb[FR][:, 0:512], Fb[FR][:, 0:512], AF.Exp, [("F", FR)], [("F", FR)], scale=-0.5)
                tt(Hb[TT_][:, :], PS[bo][:, :], Fb[FR][:, 0:512], ALU.mult, [("ps", bo), ("F", FR)], [("H", TT_)])
                tt(BR[:, :, tok], v3(Hb[TT_][:, :], 4), BR[:, :, tok], ALU.mult, [("H", TT_), ("BR", st)], [("BR", st)])

        def out_stage(bi):
            s = next_slot()
            Wgt = SLOT[s][:, 0:NK * D].rearrange("p (k c) -> p k c", k=NK)
            c0 = 4112 + bi * 1024
            load_group(s, [(Wgt[:, k, :], w_in_d[l, k * 128:(k + 1) * 128, c0:c0 + 1024]) for k in range(NK)])
            dma("pool", [(SLW[:, kc, :], wA_d[bi][l, kc * 128:(kc + 1) * 128, :]) for kc in range(4)], ["SLW"], "slw")
            j = 0
            for st in range(NST):
                tsl = slice(st * 512, (st + 1) * 512)
                for dc in range(NK):
                    dsl = slice(dc * 128, (dc + 1) * 128)
                    by = nb()
                    mm(by, PS[by][:, :], [(SLW[:, kc, dsl], BR[:, kc, tsl]) for kc in range(4)], ["SLW", ("BR", st)])
                    bg = nb()
                    mm(bg, PS[bg][:, :], [(Wgt[:, k, dsl], hT[:, k, tsl]) for k in range(NK)], [("slot", s), ("hT", st)])
                    hb = 8 + (j % 2)
                    j += 1
                    act(Hb[hb][:, :], PS[bg][:, :], AF.Sigmoid, [("ps", bg)], [("H", hb)])
                    tt(Hb[dc][:, :], PS[by][:, :], Hb[hb][:, :], ALU.mult, [("ps", by), ("H", hb)], [("H", dc)])
                for dc in range(NK):
                    dsl = slice(dc * 128, (dc + 1) * 128)
                    b = nb()
                    mm(b, PS[b][:, :], [(WO[:, k, dsl], Hb[k][:, :]) for k in range(NK)], ["WO"] + [("H", k) for k in range(NK)])
                    tt(xT[:, dc, tsl], xT[:, dc, tsl], PS[b][:, :], ALU.add, [("xT", dc, st), ("ps", b)], [("xT", dc, st)])

        if STAGE < 4:
            continue
        out_stage(0)
        if STAGE < 5:
            continue

        s = next_slot()
        Ws = SLOT[s][:, 0:NK * 1024].rearrange("p (k c) -> p k c", k=NK)
        load_group(s, [(Ws[:, k, :], w_in_d[l, k * 128:(k + 1) * 128, 1552:2576]) for k in range(NK)])
        slr = ("slot", s)
        for st in range(NST):
            tsl = slice(st * 512, (st + 1) * 512)
            hres = ("hT", st)
            for c in range(4):
                b = nb()
                mm(b, PS[b][:, :], [(Ws[:, k, c * 128:(c + 1) * 128], hT[:, k, tsl]) for k in range(NK)], [slr, hres])
                act(Hb[c][:, :], PS[b][:, :], AF.Gelu_apprx_tanh, [("ps", b)], [("H", c)])
            for t4 in range(4):
                t0 = st * 512 + t4 * 128
                tok = slice(t0, t0 + 128)
                loc = slice(t4 * 128, (t4 + 1) * 128)
                p_ = t4 % 2
                f0_, f1_, f2_, h4_ = 3 * p_, 3 * p_ + 1, 3 * p_ + 2, 4 + p_
                STAT, MV = STATS[p_], MVS[p_]
                b = nb()
                mm(b, PS[b][:, :], [(hT[:, k, tok], Ws[:, k, 512:1024]) for k in range(NK)], [slr, hres])
                act(Fb[f0_][:, 0:512], PS[b][:, :], AF.Gelu_apprx_tanh, [("ps", b)], [("F", f0_)])
                S.add("dve", lambda e, STAT=STAT, f0_=f0_: e.bn_stats(out=STAT[:, 0:6], in_=Fb[f0_][:, 0:512]),
                      reads=[("F", f0_)], writes=[("STAT", p_)])
                S.add("dve", lambda e, STAT=STAT, MV=MV: e.bn_aggr(out=MV[:, 0:2], in_=STAT[:, 0:6]),
                      reads=[("STAT", p_)], writes=[("MV", p_)])
                act(MV[:, 2:3], MV[:, 1:2], AF.Ln, [("MV", p_), "eps"], [("MV2", p_)], bias=epsc[:, 0:1], scale=1.0)
                act(MV[:, 3:4], MV[:, 2:3], AF.Exp, [("MV2", p_)], [("MV3", p_)], scale=-0.5)
                ts(Fb[f1_][:, 0:512], Fb[f0_][:, 0:512], MV[:, 0:1], MV[:, 3:4], ALU.subtract, ALU.mult,
                   [("F", f0_), ("MV", p_), ("MV3", p_)], [("F", f1_)])
                tt(Fb[f1_][:, 0:512], Fb[f1_][:, 0:512], LNG[:, :], ALU.mult, [("F", f1_), "LNG"], [("F", f1_)])
                tt(Hb[h4_][:, :], Fb[f1_][:, 0:512], LNB[:, :], ALU.add, [("F", f1_), "LNB"], [("H", h4_)])
                bm = nb()
                for h in range(4):
                    hs = slice(h * 128, (h + 1) * 128)
                    mm(bm, PS[bm][:, hs], [(Hb[h4_][:, hs], WST[:, hs])], [("H", h4_), "WST"])
                tt(Fb[f2_][:, 0:512], PS[bm][:, :], BSB[:, :], ALU.add, [("ps", bm), "BSB"], [("F", f2_)])
                for h in range(4):
                    hs = slice(h * 128, (h + 1) * 128)
                    tt(BR[:, h, tok], Fb[f2_][:, hs], Hb[h][:, loc], ALU.mult, [("F", f2_), ("H", h)], [("BR", st)])
        if STAGE < 6:
            continue
        out_stage(1)
        if STAGE < 7:
            continue

        for cpair in range(2):
            s = next_slot()
            Wc = SLOT[s][:, 0:NK * 768].rearrange("p (k c) -> p k c", k=NK)
            prs = []
            for k in range(NK):
                for j3 in range(3):
                    c0 = 2576 + j3 * 512 + cpair * 256
                    prs.append((Wc[:, k, j3 * 256:(j3 + 1) * 256], w_in_d[l, k * 128:(k + 1) * 128, c0:c0 + 256]))
            load_group(s, prs)
            slr = ("slot", s)
            for ci in range(2):
                c = cpair * 2 + ci
                for st in range(NST):
                    tsl = slice(st * 512, (st + 1) * 512)
                    hres = ("hT", st)
                    pp = st % 2
                    bx, bc_, bb = nb(), nb(), nb()
                    mm(bx, PS[bx][:, :], [(Wc[:, k, 512 + ci * 128:512 + (ci + 1) * 128], hT[:, k, tsl]) for k in range(NK)], [slr, hres])
                    mm(bc_, PS[bc_][:, :], [(Wc[:, k, 256 + ci * 128:256 + (ci + 1) * 128], hT[:, k, tsl]) for k in range(NK)], [slr, hres])
                    mm(bb, PS[bb][:, :], [(Wc[:, k, ci * 128:(ci + 1) * 128], hT[:, k, tsl]) for k in range(NK)], [slr, hres])
                    act(Fb[2][:, 0:512], PS[bx][:, :], AF.Copy, [("ps", bx)], [("F", 2)])
                    if st == 0:
                        ms("dve", Fb[pp][:, 0:2], 0.0, [("F", pp)])
                    else:
                        cp("act", Fb[pp][:, 0:2], Fb[1 - pp][:, 512:514], [("F", 1 - pp)], [("F", pp)])
                    tt(Fb[pp][:, 2:514], PS[bc_][:, :], Fb[2][:, 0:512], ALU.mult, [("ps", bc_), ("F", 2)], [("F", pp)])
                    cw = lambda kk: cvl[:, 20 + kk * 4 + c:21 + kk * 4 + c]
                    ts(Fb[3][:, 0:512], Fb[pp][:, 2:514], cw(2), None, ALU.mult, None, [("F", pp), "cvl"], [("F", 3)])
                    stt(Fb[3][:, 0:512], Fb[pp][:, 1:513], cw(1), Fb[3][:, 0:512], ALU.mult, ALU.add, [("F", pp), ("F", 3), "cvl"], [("F", 3)])
                    stt(Fb[3][:, 0:512], Fb[pp][:, 0:512], cw(0), Fb[3][:, 0:512], ALU.mult, ALU.add, [("F", pp), ("F", 3), "cvl"], [("F", 3)])
                    tt(BR[:, c, tsl], Fb[3][:, 0:512], PS[bb][:, :], ALU.mult, [("F", 3), ("ps", bb)], [("BR", st)])
        if STAGE < 8:
            continue
        out_stage(2)
        if STAGE < 9:
            continue

        norm_to_h(8)
        f0 = 0
        while f0 < NF:
            G = min(4, NF - f0)
            s = next_slot()
            Wu = SLOT[s][:, 0:NK * 1024].rearrange("p (k c) -> p k c", k=NK)
            prs = []
            for k in range(NK):
                prs.append((Wu[:, k, 0:G * 128], w_up_d[l, k * 128:(k + 1) * 128, f0 * 128:(f0 + G) * 128]))
                prs.append((Wu[:, k, 512:512 + G * 128], w_up_d[l, k * 128:(k + 1) * 128, DFF + f0 * 128:DFF + (f0 + G) * 128]))
            load_group(s, prs)
            dma("pool", [(SLW[:, j, :], w_dn_d[l, (f0 + j) * 128:(f0 + j + 1) * 128, :]) for j in range(G)], ["SLW"], "slw")
            slr = ("slot", s)
            for j in range(G):
                f = f0 + j
                cg = lambda kk: cvl[:, 32 + kk * 44 + f:33 + kk * 44 + f]
                cvv = lambda kk: cvl[:, 32 + kk * 44 + 22 + f:33 + kk * 44 + 22 + f]
                bgc = cvl[:, 164 + f:165 + f]
                bvc = cvl[:, 164 + 22 + f:165 + 22 + f]
                for st in range(NST):
                    tsl = slice(st * 512, (st + 1) * 512)
                    hres = ("hT", st)
                    pp = st % 2
                    ug, uv = pp, 2 + pp
                    bg, bv = nb(), nb()
                    mm(bg, PS[bg][:, :], [(Wu[:, k, j * 128:(j + 1) * 128], hT[:, k, tsl]) for k in range(NK)], [slr, hres])
                    mm(bv, PS[bv][:, :], [(Wu[:, k, 512 + j * 128:512 + (j + 1) * 128], hT[:, k, tsl]) for k in range(NK)], [slr, hres])
                    for (u, b_) in ((ug, bg), (uv, bv)):
                        if st == 0:
                            ms("dve", Fb[u][:, 0:2], 0.0, [("F", u)])
                        else:
                            uo = u - pp + (1 - pp)
                            cp("act", Fb[u][:, 0:2], Fb[uo][:, 512:514], [("F", uo)], [("F", u)])
                        act(Fb[u][:, 2:514], PS[b_][:, :], AF.Copy, [("ps", b_)], [("F", u)])
                    for (u, dst, cw_, bc_, b_) in ((ug, 4, cg, bgc, bg), (uv, 5, cvv, bvc, bv)):
                        act(Fb[dst][:, 0:512], PS[b_][:, :], AF.Identity, [("ps", b_), "cvl"], [("F", dst)],
                            bias=bc_, scale=cw_(2))
                        stt(Fb[dst][:, 0:512], Fb[u][:, 1:513], cw_(1), Fb[dst][:, 0:512], ALU.mult, ALU.add,
                            [("F", u), ("F", dst), "cvl"], [("F", dst)])
                        stt(Fb[dst][:, 0:512], Fb[u][:, 0:512], cw_(0), Fb[dst][:, 0:512], ALU.mult, ALU.add,
                            [("F", u), ("F", dst), "cvl"], [("F", dst)])
                    hb = 8 + ((j * NST + st) % 2)
                    act(Hb[hb][:, :], Fb[4][:, 0:512], AF.Silu, [("F", 4)], [("H", hb)])
                    tt(BR[:, j, tsl], Hb[hb][:, :], Fb[5][:, 0:512], ALU.mult, [("H", hb), ("F", 5)], [("BR", st)])
            for st in range(NST):
                tsl = slice(st * 512, (st + 1) * 512)
                for dc in range(NK):
                    dsl = slice(dc * 128, (dc + 1) * 128)
                    b = nb()
                    mm(b, PS[b][:, :], [(SLW[:, j, dsl], BR[:, j, tsl]) for j in range(G)], ["SLW", ("BR", st)])
                    tt(xT[:, dc, tsl], xT[:, dc, tsl], PS[b][:, :], ALU.add, [("xT", dc, st), ("ps", b)], [("xT", dc, st)])
            f0 += G

    finals = []
    for st in range(NST):
        tsl = slice(st * 512, (st + 1) * 512)
        rmsnorm_rstd(st, tsl)
        for k in range(NK):
            ob = 1 + (k % 4)
            stt(Fb[ob][:, 0:512], xT[:, k, tsl], cvf[:, k:k + 1], Fb[0][:, 0:512], ALU.mult, ALU.mult,
                [("xT", k, st), ("F", 0), "cvf"], [("F", ob)])
            finals.append(dma("sp", [(yT_d[k * 128:(k + 1) * 128, tsl], Fb[ob][:, 0:512])], [], "out%d" % ob,
                              reads=[("F", ob)]))
    S.emit(nc, es, final_waits=finals)
    es.close()
    return nc


def prep_inputs(inp, NL=4):
    f = lambda a: np.ascontiguousarray(np.asarray(a, dtype=np.float32))
    sh = {}
    sh["w_in"] = f(inp["w_in"][:NL])
    sh["gla_w_out"] = f(inp["gla_w_out"][:NL])
    sh["sgu_w_out"] = f(inp["sgu_w_out"][:NL])
    sh["conv_w_out"] = f(inp["conv_w_out"][:NL])
    sh["w_o"] = f(inp["w_o"][:NL])
    sh["ffn_w_up"] = f(inp["ffn_w_up"][:NL])
    sh["ffn_w_down"] = f(inp["ffn_w_down"][:NL])
    sh["walpha17"] = f(np.concatenate([np.asarray(inp["gla_w_alpha"])[:NL], np.asarray(inp["gla_b_alpha"])[:NL, None, :]], axis=1))
    sh["wsT"] = f(np.transpose(np.asarray(inp["sgu_ws"])[:NL], (0, 3, 1, 2)).reshape(NL, 128, 512))
    cv = np.zeros((128, NL * CV_L + 8), np.float32)
    pm = lambda v, n: np.asarray(v, dtype=np.float32).reshape(n, 128).T
    for l in range(NL):
        b = l * CV_L
        cv[:, b:b + 8] = pm(inp["norm_mix"][l], 8)
        cv[:, b + 8:b + 16] = pm(inp["norm_ffn"][l], 8)
        cv[:, b + 16:b + 20] = pm(inp["gla_norm"][l], 4)
        for kk in range(3):
            cv[:, b + 20 + kk * 4:b + 24 + kk * 4] = pm(inp["conv_w"][l][kk], 4)
            cv[:, b + 32 + kk * 44:b + 32 + (kk + 1) * 44] = pm(inp["ffn_conv_w"][l][kk], 44)
        cv[:, b + 164:b + 208] = pm(inp["ffn_conv_b"][l], 44)
    cv[:, NL * CV_L:NL * CV_L + 8] = pm(inp["norm_final"], 8)
    sh["cvec"] = cv
    rb = np.zeros((NL, 128, 1536), np.float32)
    for l in range(NL):
        rb[l, :, 0:512] = np.asarray(inp["sgu_ln_g"])[l][None, :]
        rb[l, :, 512:1024] = np.asarray(inp["sgu_ln_b"])[l][None, :]
        rb[l, :, 1024:1536] = np.asarray(inp["sgu_bs"])[l].reshape(1, 512)
    sh["rowb"] = rb
    j = np.arange(128)[:, None]
    i = np.arange(128)[None, :]
    cm = np.zeros((128, 768), np.float32)
    cm[:, 0:128] = np.where(j <= i, -1.0 / 16.0, 0.0)
    cm[:, 128:256] = np.where(j > i, -1.0 / 16.0, 0.0)
    cm[:, 256:768] = np.tile(np.where(j <= i, 1.0, 0.0), (1, 4))
    sh["cmat"] = cm
    return sh


_CACHE = {}


def run(inputs, T, NL, ncores):
    key = (T, NL)
    if key not in _CACHE:
        _CACHE[key] = build(T, NL)
    nc = _CACHE[key]
    shared = prep_inputs(inputs, NL)
    x = np.asarray(inputs["x"], dtype=np.float32)
    in_maps = []
    for c in range(ncores):
        m = dict(shared)
        m["xT"] = np.ascontiguousarray(x[c, :T].T)
        in_maps.append(m)
    res = run_bass_kernel_spmd(nc, in_maps, core_ids=list(range(ncores)))
    out = np.stack([np.ascontiguousarray(r["yT"].T) for r in res.results], axis=0)
    return out.astype(np.float32)


def kernel(**inputs):
    return run(inputs, 2048, 4, 8)
```

```python
from contextlib import ExitStack
import numpy as np
import concourse.bass as bass
import concourse.mybir as mybir
from concourse.bass_utils import run_bass_kernel_spmd

F32 = mybir.dt.float32
BF16 = mybir.dt.bfloat16
AF = mybir.ActivationFunctionType
ALU = mybir.AluOpType

D = 1024
NK = 8
INC = 7184
DFF = 2816
NF = 22
EPS = 1e-6
CV_L = 208


class _Op:
    __slots__ = ("eng", "fn", "deps", "dma", "ndma", "signal", "count", "sem")


class Sched:
    ENGS = ("pe", "act", "dve", "pool", "sp")

    def __init__(self):
        self.ops = []
        self.by_eng = {e: [] for e in self.ENGS}
        self.last_w = {}
        self.readers = {}

    def add(self, eng, fn, reads=(), writes=(), dma=None, ndma=1):
        op = _Op()
        op.eng, op.fn, op.dma, op.ndma = eng, fn, dma, ndma
        op.signal = dma is not None
        op.count = 0
        op.sem = None
        deps = {}

        def need(d, raw):
            if d.dma is not None or dma is not None:
                return True
            if d.eng != eng:
                return True
            if eng == "pe":
                return False
            return True

        for r in reads:
            w = self.last_w.get(r)
            if w is not None and need(w, True):
                deps[id(w)] = w
        for r in writes:
            w = self.last_w.get(r)
            if w is not None and need(w, False):
                deps[id(w)] = w
            for rd in self.readers.get(r, {}).values():
                if rd is not op and need(rd, False):
                    deps[id(rd)] = rd
        op.deps = list(deps.values())
        for d in op.deps:
            d.signal = True
        for r in reads:
            self.readers.setdefault(r, {})[eng if dma is None else ("dma", len(self.ops))] = op
        for r in writes:
            self.last_w[r] = op
            self.readers[r] = {}
        self.ops.append(op)
        self.by_eng[eng].append(op)
        return op

    def emit(self, nc, es, final_waits=()):
        sems = {}

        def get_sem(key):
            if key not in sems:
                sems[key] = es.enter_context(nc.semaphore("s_%s_%s" % key))
            return sems[key]

        cnt = {}
        for op in self.ops:
            if op.dma is not None:
                key = ("d", op.dma)
                cnt[key] = cnt.get(key, 0) + 16 * op.ndma
                op.sem, op.count = key, cnt[key]
            elif op.signal:
                key = ("e", op.eng)
                cnt[key] = cnt.get(key, 0) + 1
                op.sem, op.count = key, cnt[key]
        for k in cnt:
            get_sem(k)
        block = es.enter_context(nc.Block())
        sections = {"pe": block.tensor, "act": block.scalar, "dve": block.vector,
                    "pool": block.gpsimd, "sp": block.sync}

        def make(engname):
            oplist = self.by_eng[engname]

            def body(e):
                waited = {}
                for op in oplist:
                    need = {}
                    for d in op.deps:
                        if need.get(d.sem, 0) < d.count:
                            need[d.sem] = d.count
                    for k, v in need.items():
                        if waited.get(k, 0) < v:
                            e.wait_ge(sems[k], v)
                            waited[k] = v
                    if op.dma is not None:
                        op.fn(e, sems[op.sem])
                    else:
                        ins = op.fn(e)
                        if op.signal:
                            ins.then_inc(sems[op.sem], 1)
                if engname == "sp":
                    for op in final_waits:
                        e.wait_ge(sems[op.sem], op.count)
            return body

        for en in self.ENGS:
            sections[en](make(en))


STAGE = 99
SUB = 99


def build(T=2048, NL=4):
    NST = T // 512
    GST = 256
    nc = bass.Bass("TRN2", target_bir_lowering=False)
    es = ExitStack()

    def dram(name, shape, kind="ExternalInput"):
        return nc.dram_tensor(name, list(shape), F32, kind=kind).ap()

    xT_d = dram("xT", [D, T])
    w_in_d = dram("w_in", [NL, D, INC])
    wA_d = [dram(n, [NL, 512, D]) for n in ("gla_w_out", "sgu_w_out", "conv_w_out")]
    w_o_d = dram("w_o", [NL, D, D])
    w_up_d = dram("ffn_w_up", [NL, D, 2 * DFF])
    w_dn_d = dram("ffn_w_down", [NL, DFF, D])
    walpha_d = dram("walpha17", [NL, 17, 256])
    wsT_d = dram("wsT", [NL, 128, 512])
    cvec_d = dram("cvec", [128, NL * CV_L + 8])
    rowb_d = dram("rowb", [NL, 128, 1536])
    cmat_d = dram("cmat", [128, 768])
    yT_d = dram("yT", [D, T], kind="ExternalOutput")

    def sb(name, shape, dt):
        return es.enter_context(nc.sbuf_tensor(name, list(shape), dt))

    xT = sb("xT_s", [128, NK, T], F32)
    hT = sb("hT_s", [128, NK, T], BF16)
    BR = sb("BR_s", [128, 4, T], BF16)
    WO = sb("WO_s", [128, NK, D], BF16)
    SLOT_E = 8320
    SLOT = [sb("slot%d" % i, [128, SLOT_E], BF16) for i in range(2)]
    SLW = sb("slotW", [128, 4, D], BF16)
    NFB, NHB = 6, 16
    Fb = [sb("F%d" % i, [128, 516], F32) for i in range(NFB)]
    Hb = [sb("H%d" % i, [128, 512], BF16) for i in range(NHB)]
    cvf = sb("cvf", [128, 8], F32)
    cvl = sb("cvl", [128, CV_L], F32)
    triU = sb("triU", [128, 128], BF16)
    triSU = sb("triSU", [128, 128], BF16)
    MASK = sb("MASK", [128, 512], BF16)
    ones_bf = sb("ones_bf", [128, 128], BF16)
    epsc = sb("epsc", [128, 1], F32)
    A17 = sb("A17", [32, GST], BF16)
    walpha = sb("walpha", [32, 256], BF16)
    WST = sb("WST", [128, 512], BF16)
    LNG = sb("LNG", [128, 512], BF16)
    LNB = sb("LNB", [128, 512], BF16)
    BSB = sb("BSB", [128, 512], F32)
    Sst = sb("Sst", [128, 256], F32)
    Sbf = sb("Sbf", [128, 256], BF16)
    DEC = sb("DEC", [128, 2], F32)
    STATS = [sb("STAT%d" % i, [128, 8], F32) for i in range(2)]
    MVS = [sb("MV%d" % i, [128, 4], F32) for i in range(2)]
    PS = [es.enter_context(nc.psum_tensor("ps%d" % i, [128, 512], F32)) for i in range(8)]

    S = Sched()
    bank_ctr = [0]

    def nb():
        b = bank_ctr[0] % 8
        bank_ctr[0] += 1
        return b

    def mm(bank, out_ap, pairs, reads):
        def fn(pe):
            n = len(pairs)
            ins = None
            for i, (l, r) in enumerate(pairs):
                ins = pe.matmul(out_ap, l, r, start=(i == 0), stop=(i == n - 1))
            return ins
        return S.add("pe", fn, reads=reads, writes=[("ps", bank)])

    def act(out, in_, func, reads, writes, bias=None, scale=None):
        kw = {}
        if bias is not None:
            kw["bias"] = bias
        if scale is not None:
            kw["scale"] = scale
        return S.add("act", lambda e: e.activation(out=out, in_=in_, func=func, **kw), reads=reads, writes=writes)

    def tt(out, in0, in1, op, reads, writes, eng="dve"):
        return S.add(eng, lambda e: e.tensor_tensor(out=out, in0=in0, in1=in1, op=op), reads=reads, writes=writes)

    def ts(out, in0, s1, s2, op0, op1, reads, writes, eng="dve"):
        if s2 is None:
            return S.add(eng, lambda e: e.tensor_scalar(out=out, in0=in0, scalar1=s1, scalar2=None, op0=op0),
                         reads=reads, writes=writes)
        return S.add(eng, lambda e: e.tensor_scalar(out=out, in0=in0, scalar1=s1, scalar2=s2, op0=op0, op1=op1),
                     reads=reads, writes=writes)

    def stt(out, in0, scalar, in1, op0, op1, reads, writes, eng="dve"):
        return S.add(eng, lambda e: e.scalar_tensor_tensor(out=out, in0=in0, scalar=scalar, in1=in1, op0=op0, op1=op1),
                     reads=reads, writes=writes)

    def cp(eng, out, in_, reads, writes):
        if eng == "act":
            return S.add(eng, lambda e: e.activation(out=out, in_=in_, func=AF.Copy), reads=reads, writes=writes)
        return S.add(eng, lambda e: e.tensor_copy(out=out, in_=in_), reads=reads, writes=writes)

    def ms(eng, ap, val, writes):
        return S.add(eng, lambda e: e.memset(ap, val), reads=[], writes=writes)

    def dma(eng, pairs, writes, semkey, reads=()):
        def fn(e, sem):
            for o, i in pairs:
                e.dma_start(out=o, in_=i).then_inc(sem, 16)
        return S.add(eng, fn, reads=reads, writes=writes, dma=semkey, ndma=len(pairs))

    def v3(ap, a):
        return ap.rearrange("p (a b) -> p a b", a=a)

    dma("sp", [(xT[:, k, :], xT_d[k * 128:(k + 1) * 128, :]) for k in range(NK)],
        [("xT", k, st) for k in range(NK) for st in range(NST)], "init")
    dma("sp", [(cvf[:, :], cvec_d[:, NL * CV_L:NL * CV_L + 8])], ["cvf"], "init2")
    dma("pool", [(MASK[:, :], cmat_d[:, 256:768]), (triU[:, :], cmat_d[:, 0:128]), (triSU[:, :], cmat_d[:, 128:256])],
        ["MASK", "tri"], "initp")
    ms("dve", ones_bf[:, :], 1.0, ["ones"])
    ms("dve", epsc[:, :], EPS, ["eps"])
    ms("dve", A17[:, :], 1.0, ["A17"])

    def rmsnorm_rstd(st, tsl):
        b = nb()
        sqs = []
        for k in range(NK):
            hb = Hb[8 + (k % 2)]
            hres = ("H", 8 + (k % 2))
            act(hb[:, :], xT[:, k, tsl], AF.Square, [("xT", k, st)], [hres])
            sqs.append((hb, hres))
            def fn(pe, k=k, hb=hb, b=b):
                return pe.matmul(PS[b][:, :], ones_bf[:, :], hb[:, :], start=(k == 0), stop=(k == NK - 1))
            S.add("pe", fn, reads=[hres, "ones"], writes=[("ps", b)])
        act(Fb[0][:, 0:512], PS[b][:, :], AF.Ln, [("ps", b), "eps"], [("F", 0)], bias=epsc[:, 0:1], scale=1.0 / D)
        act(Fb[0][:, 0:512], Fb[0][:, 0:512], AF.Exp, [("F", 0)], [("F", 0)], scale=-0.5)
        return 0

    def norm_to_h(l_col):
        for st in range(NST):
            tsl = slice(st * 512, (st + 1) * 512)
            rmsnorm_rstd(st, tsl)
            for k in range(NK):
                stt(hT[:, k, tsl], xT[:, k, tsl], cvl[:, l_col + k:l_col + k + 1], Fb[0][:, 0:512],
                    ALU.mult, ALU.mult, [("xT", k, st), ("F", 0), "cvl"], [("hT", st)])

    slot_ctr = [0]

    def next_slot():
        s = slot_ctr[0] % 2
        slot_ctr[0] += 1
        return s

    def load_group(s, pairs):
        dma("pool", pairs, [("slot", s)], "slot%d" % s)

    for l in range(NL):
        dma("sp", [(cvl[:, :], cvec_d[:, l * CV_L:(l + 1) * CV_L]),
                   (BSB[:, :], rowb_d[l, :, 1024:1536])], ["cvl", "BSB"], "lsmall")
        dma("pool", [(WST[:, :], wsT_d[l]), (LNG[:, :], rowb_d[l, :, 0:512]), (LNB[:, :], rowb_d[l, :, 512:1024]),
                     (walpha[0:17, :], walpha_d[l])],
            ["WST", "LNG", "LNB", "walpha"], "lsmallp")
        tt(WST[:, :], WST[:, :], MASK[:, :], ALU.mult, ["WST", "MASK"], ["WST"])
        dma("pool", [(WO[:, k, :], w_o_d[l, k * 128:(k + 1) * 128, :]) for k in range(NK)], ["WO"], "wo")

        if STAGE < 1:
            continue
        norm_to_h(0)
        if STAGE < 2:
            continue

        s = next_slot()
        Wg = SLOT[s][:, 0:NK * 512].rearrange("p (k c) -> p k c", k=NK)
        load_group(s, [(Wg[:, k, :], w_in_d[l, k * 128:(k + 1) * 128, 1024:1536]) for k in range(NK)])
        i = 0
        for st in range(NST):
            tsl = slice(st * 512, (st + 1) * 512)
            for h in range(4):
                b = nb()
                mm(b, PS[b][:, :], [(Wg[:, k, h * 128:(h + 1) * 128], hT[:, k, tsl]) for k in range(NK)],
                   [("slot", s), ("hT", st)])
                hb = 8 + (i % 2)
                i += 1
                act(Hb[hb][:, :], PS[b][:, :], AF.Sigmoid, [("ps", b)], [("H", hb)])
                stt(BR[:, h, tsl], PS[b][:, :], cvl[:, 16 + h:17 + h], Hb[hb][:, :], ALU.mult, ALU.mult,
                    [("ps", b), ("H", hb), "cvl"], [("BR", st)])

        if STAGE < 3:
            continue
        s = next_slot()
        Wm = SLOT[s][:, 0:NK * 1040].rearrange("p (k c) -> p k c", k=NK)
        prs = []
        for k in range(NK):
            prs.append((Wm[:, k, 0:1024], w_in_d[l, k * 128:(k + 1) * 128, 0:1024]))
            prs.append((Wm[:, k, 1024:1040], w_in_d[l, k * 128:(k + 1) * 128, 1536:1552]))
        load_group(s, prs)
        slr = ("slot", s)
        ms("dve", Sst[:, :], 0.0, ["S"])
        ms("dve", Sbf[:, :], 0.0, ["Sbf"])
        EB, ENB, QT, KT, Vb, KEb, STM, OSQ, TT_ = 0, 1, 2, 3, 4, 5, 6, 7, 8
        QTO = 10
        ms("dve", Hb[QT][:, :], 0.0, [("H", QT)])
        ms("dve", Hb[QTO][:, :], 0.0, [("H", QTO)])
        FELA, FR = 1, 2
        for gst in range(T // GST):
            g0 = gst * GST
            gsl = slice(g0, g0 + GST)
            st = g0 // 512
            hres = ("hT", st)
            b = nb()
            mm(b, PS[b][0:16, 0:GST], [(Wm[:, k, 1024:1040], hT[:, k, gsl]) for k in range(NK)], [slr, hres])
            if SUB < 1:
                continue
            act(A17[0:16, :], PS[b][0:16, 0:GST], AF.Copy, [("ps", b)], ["A17"])
            if SUB < 2:
                continue
            for t2 in range(GST // 128):
                loc = slice(t2 * 128, (t2 + 1) * 128)
                b = nb()
                mm(b, PS[b][:, 0:256], [(A17[0:17, loc], walpha[0:17, :])], ["A17", "walpha"])
                act(Fb[FELA][:, 0:256], PS[b][:, 0:256], AF.Exp, [("ps", b)], [("F", FELA)], scale=-1.0)
                act(Fb[FELA][:, 256:512], Fb[FELA][:, 0:256], AF.Ln, [("F", FELA)], [("F", FELA)], bias=1.0, scale=1.0)
                la = Fb[FELA]
                LAH, LAL = Hb[9][:, 0:256], Hb[9][:, 256:512]
                cp("dve", LAH, la[:, 256:512], [("F", FELA)], [("H", 9)])
                tt(LAL, la[:, 256:512], LAH, ALU.subtract, [("F", FELA), ("H", 9)], [("H", 9)])
                if SUB < 3:
                    continue
                b = nb()
                for cc in range(2):
                    mm(b, PS[b][:, cc * 128:(cc + 1) * 128],
                       [(LAH[:, cc * 128:(cc + 1) * 128], triU[:, :]), (LAL[:, cc * 128:(cc + 1) * 128], triU[:, :])],
                       [("H", 9), "tri"])
                mm(b, PS[b][:, 256:512], [(triSU[:, :], LAH), (triSU[:, :], LAL)], [("H", 9), "tri"])
                for cc in range(2):
                    act(Hb[EB][:, cc * GST + t2 * 128:cc * GST + (t2 + 1) * 128], PS[b][:, cc * 128:(cc + 1) * 128],
                        AF.Exp, [("ps", b)], [("H", EB)])
                    act(Hb[ENB][:, cc * GST + t2 * 128:cc * GST + (t2 + 1) * 128], PS[b][:, cc * 128:(cc + 1) * 128],
                        AF.Exp, [("ps", b)], [("H", ENB)], scale=-1.0)
                if SUB < 4:
                    continue
                act(Fb[3 + t2][:, 0:256], PS[b][:, 256:512], AF.Exp, [("ps", b)], [("F", 3 + t2)])
                act(Fb[3 + t2][:, 256:258], PS[b][:, 127:256:128], AF.Exp, [("ps", b)], [("F", 3 + t2)])
            if SUB < 5:
                continue
            for cc in range(2):
                b = nb()
                mm(b, PS[b][:, 0:GST], [(Wm[:, k, cc * 128:(cc + 1) * 128], hT[:, k, gsl]) for k in range(NK)], [slr, hres])
                stt(Hb[QT][0:64, cc * GST:(cc + 1) * GST], PS[b][0:64, 0:GST], 0.125, Hb[EB][0:64, cc * GST:(cc + 1) * GST],
                    ALU.mult, ALU.mult, [("ps", b), ("H", EB)], [("H", QT)])
                stt(Hb[QTO][64:128, cc * GST:(cc + 1) * GST], PS[b][64:128, 0:GST], 0.125, Hb[EB][64:128, cc * GST:(cc + 1) * GST],
                    ALU.mult, ALU.mult, [("ps", b), ("H", EB)], [("H", QTO)])
                b = nb()
                mm(b, PS[b][:, 0:GST], [(Wm[:, k, 256 + cc * 128:256 + (cc + 1) * 128], hT[:, k, gsl]) for k in range(NK)],
                   [slr, hres])
                tt(Hb[KT][:, cc * GST:(cc + 1) * GST], PS[b][:, 0:GST], Hb[ENB][:, cc * GST:(cc + 1) * GST], ALU.mult,
                   [("ps", b), ("H", ENB)], [("H", KT)])
            tstate = {}

            def gla_front(t2):
                t0 = g0 + t2 * 128
                tok = slice(t0, t0 + 128)
                fe = 3 + t2
                Vb, KEb, STM, OSQ, TT_ = ((4, 5, 6, 7, 8), (11, 12, 13, 14, 15))[t2]
                b = nb()
                mm(b, PS[b][:, :], [(hT[:, k, tok], Wm[:, k, 512:1024]) for k in range(NK)], [slr, hres])
                act(Hb[Vb][:, :], PS[b][:, :], AF.Copy, [("ps", b)], [("H", Vb)])
                b = nb()
                mm(b, PS[b][:, 0:256], [(hT[:, k, tok], Wm[:, k, 256:512]) for k in range(NK)], [slr, hres])
                tt(Hb[KEb][:, 0:256], PS[b][:, 0:256], Fb[fe][:, 0:256], ALU.mult, [("ps", b), ("F", fe)], [("H", KEb)])
                b = nb()
                for h in range(4):
                    cc = h // 2
                    c0 = cc * GST + t2 * 128
                    qz = QT if h % 2 == 0 else QTO
                    mm(b, PS[b][:, h * 128:(h + 1) * 128],
                       [(Hb[KT][:, c0:c0 + 128], Hb[qz][:, c0:c0 + 128])], [("H", KT), ("H", qz)])
                tt(Hb[STM][:, :], PS[b][:, :], MASK[:, :], ALU.mult, [("ps", b), "MASK"], [("H", STM)])

            def gla_mid(t2):
                fe = 3 + t2
                Vb, KEb, STM, OSQ, TT_ = ((4, 5, 6, 7, 8), (11, 12, 13, 14, 15))[t2]
                bo = nb()
                for h in range(4):
                    cc = h // 2
                    c0 = cc * GST + t2 * 128
                    qz = QT if h % 2 == 0 else QTO
                    mm(bo, PS[bo][:, h * 128:(h + 1) * 128],
                       [(Sbf[:, cc * 128:(cc + 1) * 128], Hb[qz][:, c0:c0 + 128]),
                        (Hb[Vb][:, h * 128:(h + 1) * 128], Hb[STM][:, h * 128:(h + 1) * 128])],
                       ["Sbf", ("H", qz), ("H", Vb), ("H", STM)])
                bc = nb()
                for cc in range(2):
                    mm(bc, PS[bc][:, cc * 256:(cc + 1) * 256],
                       [(Hb[KEb][:, cc * 128:(cc + 1) * 128], Hb[Vb][:, cc * 256:(cc + 1) * 256])], [("H", KEb), ("H", Vb)])
                for h in range(4):
                    cc, po = h // 2, (h % 2) * 64
                    hf = h % 2
                    stt(Sst[po:po + 64, cc * 128:(cc + 1) * 128], Sst[po:po + 64, cc * 128:(cc + 1) * 128],
                        Fb[fe][po:po + 64, 256 + cc:257 + cc],
                        PS[bc][po:po + 64, cc * 256 + hf * 128:cc * 256 + (hf + 1) * 128],
                        ALU.mult, ALU.add, ["S", ("F", fe), ("ps", bc)], ["S"])
                act(Sbf[:, :], Sst[:, :], AF.Copy, ["S"], ["Sbf"])
                act(Hb[OSQ][:, :], PS[bo][:, :], AF.Square, [("ps", bo)], [("H", OSQ)])
                tstate[t2] = bo

            def gla_tail(t2):
                t0 = g0 + t2 * 128
                tok = slice(t0, t0 + 128)
                Vb, KEb, STM, OSQ, TT_ = ((4, 5, 6, 7, 8), (11, 12, 13, 14, 15))[t2]
                FR = (2, 5)[t2]
                bo = tstate[t2]
                bs_ = nb()
                mm(bs_, PS[bs_][:, :], [(ones_bf[:, :], Hb[OSQ][:, :])], ["ones", ("H", OSQ)])
                act(Fb[FR][:, 0:512], PS[bs_][:, :], AF.Ln, [("ps", bs_), "eps"], [("F", FR)], bias=epsc[:, 0:1], scale=1.0 / 128)
                act(Fb[FR][:, 0:512], Fb[FR][:, 0:512], AF.Exp, [("F", FR)], [("F", FR)], scale=-0.5)
                tt(Hb[TT_][:, :], PS[bo][:, :], Fb[FR][:, 0:512], ALU.mult, [("ps", bo), ("F", FR)], [("H", TT_)])
                tt(BR[:, :, tok], v3(Hb[TT_][:, :], 4), BR[:, :, tok], ALU.mult, [("H", TT_), ("BR", st)], [("BR", st)])

            gla_front(0)
            gla_front(1)
            gla_mid(0)
            gla_mid(1)
            gla_tail(0)
            gla_tail(1)

        def out_stage(bi):
            s = next_slot()
            Wgt = SLOT[s][:, 0:NK * D].rearrange("p (k c) -> p k c", k=NK)
            c0 = 4112 + bi * 1024
            load_group(s, [(Wgt[:, k, :], w_in_d[l, k * 128:(k + 1) * 128, c0:c0 + 1024]) for k in range(NK)])
            dma("pool", [(SLW[:, kc, :], wA_d[bi][l, kc * 128:(kc + 1) * 128, :]) for kc in range(4)], ["SLW"], "slw")
            j = 0
            for st in range(NST):
                tsl = slice(st * 512, (st + 1) * 512)
                for dc in range(NK):
                    dsl = slice(dc * 128, (dc + 1) * 128)
                    by = nb()
                    mm(by, PS[by][:, :], [(SLW[:, kc, dsl], BR[:, kc, tsl]) for kc in range(4)], ["SLW", ("BR", st)])
                    bg = nb()
                    mm(bg, PS[bg][:, :], [(Wgt[:, k, dsl], hT[:, k, tsl]) for k in range(NK)], [("slot", s), ("hT", st)])
                    hb = 8 + (j % 2)
                    j += 1
                    act(Hb[hb][:, :], PS[bg][:, :], AF.Sigmoid, [("ps", bg)], [("H", hb)])
                    tt(Hb[dc][:, :], PS[by][:, :], Hb[hb][:, :], ALU.mult, [("ps", by), ("H", hb)], [("H", dc)])
                for dc in range(NK):
                    dsl = slice(dc * 128, (dc + 1) * 128)
                    b = nb()
                    mm(b, PS[b][:, :], [(WO[:, k, dsl], Hb[k][:, :]) for k in range(NK)], ["WO"] + [("H", k) for k in range(NK)])
                    tt(xT[:, dc, tsl], xT[:, dc, tsl], PS[b][:, :], ALU.add, [("xT", dc, st), ("ps", b)], [("xT", dc, st)])

        if STAGE < 4:
            continue
        out_stage(0)
        if STAGE < 5:
            continue

        s = next_slot()
        Ws = SLOT[s][:, 0:NK * 1024].rearrange("p (k c) -> p k c", k=NK)
        load_group(s, [(Ws[:, k, :], w_in_d[l, k * 128:(k + 1) * 128, 1552:2576]) for k in range(NK)])
        slr = ("slot", s)
        for st in range(NST):
            tsl = slice(st * 512, (st + 1) * 512)
            hres = ("hT", st)
            for c in range(4):
                b = nb()
                mm(b, PS[b][:, :], [(Ws[:, k, c * 128:(c + 1) * 128], hT[:, k, tsl]) for k in range(NK)], [slr, hres])
                act(Hb[c][:, :], PS[b][:, :], AF.Gelu_apprx_tanh, [("ps", b)], [("H", c)])
            def sgu_A(t4):
                t0 = st * 512 + t4 * 128
                tok = slice(t0, t0 + 128)
                p_ = t4 % 2
                f0_, f1_, h4_ = 3 * p_, 3 * p_ + 1, 4 + p_
                STAT, MV = STATS[p_], MVS[p_]
                b = nb()
                mm(b, PS[b][:, :], [(hT[:, k, tok], Ws[:, k, 512:1024]) for k in range(NK)], [slr, hres])
                act(Fb[f0_][:, 0:512], PS[b][:, :], AF.Gelu_apprx_tanh, [("ps", b)], [("F", f0_)])
                S.add("dve", lambda e, STAT=STAT, f0_=f0_: e.bn_stats(out=STAT[:, 0:6], in_=Fb[f0_][:, 0:512]),
                      reads=[("F", f0_)], writes=[("STAT", p_)])
                S.add("dve", lambda e, STAT=STAT, MV=MV: e.bn_aggr(out=MV[:, 0:2], in_=STAT[:, 0:6]),
                      reads=[("STAT", p_)], writes=[("MV", p_)])
                act(MV[:, 2:3], MV[:, 1:2], AF.Ln, [("MV", p_), "eps"], [("MV2", p_)], bias=epsc[:, 0:1], scale=1.0)
                act(MV[:, 3:4], MV[:, 2:3], AF.Exp, [("MV2", p_)], [("MV3", p_)], scale=-0.5)
                ts(Fb[f1_][:, 0:512], Fb[f0_][:, 0:512], MV[:, 0:1], MV[:, 3:4], ALU.subtract, ALU.mult,
                   [("F", f0_), ("MV", p_), ("MV3", p_)], [("F", f1_)])
                tt(Fb[f1_][:, 0:512], Fb[f1_][:, 0:512], LNG[:, :], ALU.mult, [("F", f1_), "LNG"], [("F", f1_)])
                tt(Hb[h4_][:, :], Fb[f1_][:, 0:512], LNB[:, :], ALU.add, [("F", f1_), "LNB"], [("H", h4_)])

            def sgu_B(t4):
                t0 = st * 512 + t4 * 128
                tok = slice(t0, t0 + 128)
                loc = slice(t4 * 128, (t4 + 1) * 128)
                p_ = t4 % 2
                f2_, h4_ = 3 * p_ + 2, 4 + p_
                bm = nb()
                for h in range(4):
                    hs = slice(h * 128, (h + 1) * 128)
                    mm(bm, PS[bm][:, hs], [(Hb[h4_][:, hs], WST[:, hs])], [("H", h4_), "WST"])
                tt(Fb[f2_][:, 0:512], PS[bm][:, :], BSB[:, :], ALU.add, [("ps", bm), "BSB"], [("F", f2_)])
                for h in range(4):
                    hs = slice(h * 128, (h + 1) * 128)
                    tt(BR[:, h, tok], Fb[f2_][:, hs], Hb[h][:, loc], ALU.mult, [("F", f2_), ("H", h)], [("BR", st)])

            sgu_A(0)
            sgu_A(1)
            sgu_B(0)
            sgu_A(2)
            sgu_B(1)
            sgu_A(3)
            sgu_B(2)
            sgu_B(3)
        if STAGE < 6:
            continue
        out_stage(1)
        if STAGE < 7:
            continue

        for cpair in range(2):
            s = next_slot()
            Wc = SLOT[s][:, 0:NK * 768].rearrange("p (k c) -> p k c", k=NK)
            prs = []
            for k in range(NK):
                for j3 in range(3):
                    c0 = 2576 + j3 * 512 + cpair * 256
                    prs.append((Wc[:, k, j3 * 256:(j3 + 1) * 256], w_in_d[l, k * 128:(k + 1) * 128, c0:c0 + 256]))
            load_group(s, prs)
            slr = ("slot", s)
            for ci in range(2):
                c = cpair * 2 + ci
                for st in range(NST):
                    tsl = slice(st * 512, (st + 1) * 512)
                    hres = ("hT", st)
                    pp = st % 2
                    bx, bc_, bb = nb(), nb(), nb()
                    mm(bx, PS[bx][:, :], [(Wc[:, k, 512 + ci * 128:512 + (ci + 1) * 128], hT[:, k, tsl]) for k in range(NK)], [slr, hres])
                    mm(bc_, PS[bc_][:, :], [(Wc[:, k, 256 + ci * 128:256 + (ci + 1) * 128], hT[:, k, tsl]) for k in range(NK)], [slr, hres])
                    mm(bb, PS[bb][:, :], [(Wc[:, k, ci * 128:(ci + 1) * 128], hT[:, k, tsl]) for k in range(NK)], [slr, hres])
                    act(Fb[2][:, 0:512], PS[bx][:, :], AF.Copy, [("ps", bx)], [("F", 2)])
                    if st == 0:
                        ms("dve", Fb[pp][:, 0:2], 0.0, [("F", pp)])
                    else:
                        cp("act", Fb[pp][:, 0:2], Fb[1 - pp][:, 512:514], [("F", 1 - pp)], [("F", pp)])
                    tt(Fb[pp][:, 2:514], PS[bc_][:, :], Fb[2][:, 0:512], ALU.mult, [("ps", bc_), ("F", 2)], [("F", pp)])
                    cw = lambda kk: cvl[:, 20 + kk * 4 + c:21 + kk * 4 + c]
                    ts(Fb[3][:, 0:512], Fb[pp][:, 2:514], cw(2), None, ALU.mult, None, [("F", pp), "cvl"], [("F", 3)])
                    stt(Fb[3][:, 0:512], Fb[pp][:, 1:513], cw(1), Fb[3][:, 0:512], ALU.mult, ALU.add, [("F", pp), ("F", 3), "cvl"], [("F", 3)])
                    stt(Fb[3][:, 0:512], Fb[pp][:, 0:512], cw(0), Fb[3][:, 0:512], ALU.mult, ALU.add, [("F", pp), ("F", 3), "cvl"], [("F", 3)])
                    tt(BR[:, c, tsl], Fb[3][:, 0:512], PS[bb][:, :], ALU.mult, [("F", 3), ("ps", bb)], [("BR", st)])
        if STAGE < 8:
            continue
        out_stage(2)
        if STAGE < 9:
            continue

        norm_to_h(8)
        f0 = 0
        while f0 < NF:
            G = min(4, NF - f0)
            s = next_slot()
            Wu = SLOT[s][:, 0:NK * 1024].rearrange("p (k c) -> p k c", k=NK)
            prs = []
            for k in range(NK):
                prs.append((Wu[:, k, 0:G * 128], w_up_d[l, k * 128:(k + 1) * 128, f0 * 128:(f0 + G) * 128]))
                prs.append((Wu[:, k, 512:512 + G * 128], w_up_d[l, k * 128:(k + 1) * 128, DFF + f0 * 128:DFF + (f0 + G) * 128]))
            load_group(s, prs)
            dma("pool", [(SLW[:, j, :], w_dn_d[l, (f0 + j) * 128:(f0 + j + 1) * 128, :]) for j in range(G)], ["SLW"], "slw")
            slr = ("slot", s)
            for j in range(G):
                f = f0 + j
                cg = lambda kk: cvl[:, 32 + kk * 44 + f:33 + kk * 44 + f]
                cvv = lambda kk: cvl[:, 32 + kk * 44 + 22 + f:33 + kk * 44 + 22 + f]
                bgc = cvl[:, 164 + f:165 + f]
                bvc = cvl[:, 164 + 22 + f:165 + 22 + f]
                for st in range(NST):
                    tsl = slice(st * 512, (st + 1) * 512)
                    hres = ("hT", st)
                    pp = st % 2
                    ug, uv = pp, 2 + pp
                    bg, bv = nb(), nb()
                    mm(bg, PS[bg][:, :], [(Wu[:, k, j * 128:(j + 1) * 128], hT[:, k, tsl]) for k in range(NK)], [slr, hres])
                    mm(bv, PS[bv][:, :], [(Wu[:, k, 512 + j * 128:512 + (j + 1) * 128], hT[:, k, tsl]) for k in range(NK)], [slr, hres])
                    for (u, b_, dst, cw_, bc_) in ((ug, bg, 4, cg, bgc), (uv, bv, 5, cvv, bvc)):
                        if st == 0:
                            ms("dve", Fb[u][:, 0:2], 0.0, [("F", u)])
                        else:
                            uo = u - pp + (1 - pp)
                            cp("act", Fb[u][:, 0:2], Fb[uo][:, 512:514], [("F", uo)], [("F", u)])
                        act(Fb[u][:, 2:514], PS[b_][:, :], AF.Copy, [("ps", b_)], [("F", u)])
                        act(Fb[dst][:, 0:512], PS[b_][:, :], AF.Identity, [("ps", b_), "cvl"], [("F", dst)],
                            bias=bc_, scale=cw_(2))
                        stt(Fb[dst][:, 0:512], Fb[u][:, 1:513], cw_(1), Fb[dst][:, 0:512], ALU.mult, ALU.add,
                            [("F", u), ("F", dst), "cvl"], [("F", dst)])
                        stt(Fb[dst][:, 0:512], Fb[u][:, 0:512], cw_(0), Fb[dst][:, 0:512], ALU.mult, ALU.add,
                            [("F", u), ("F", dst), "cvl"], [("F", dst)])
                        if dst == 4:
                            hb = 8 + ((j * NST + st) % 2)
                            act(Hb[hb][:, :], Fb[4][:, 0:512], AF.Silu, [("F", 4)], [("H", hb)])
                    tt(BR[:, j, tsl], Hb[hb][:, :], Fb[5][:, 0:512], ALU.mult, [("H", hb), ("F", 5)], [("BR", st)])
            for st in range(NST):
                tsl = slice(st * 512, (st + 1) * 512)
                for dc in range(NK):
                    dsl = slice(dc * 128, (dc + 1) * 128)
                    b = nb()
                    mm(b, PS[b][:, :], [(SLW[:, j, dsl], BR[:, j, tsl]) for j in range(G)], ["SLW", ("BR", st)])
                    tt(xT[:, dc, tsl], xT[:, dc, tsl], PS[b][:, :], ALU.add, [("xT", dc, st), ("ps", b)], [("xT", dc, st)])
            f0 += G

    finals = []
    for st in range(NST):
        tsl = slice(st * 512, (st + 1) * 512)
        rmsnorm_rstd(st, tsl)
        for k in range(NK):
            ob = 1 + (k % 4)
            stt(Fb[ob][:, 0:512], xT[:, k, tsl], cvf[:, k:k + 1], Fb[0][:, 0:512], ALU.mult, ALU.mult,
                [("xT", k, st), ("F", 0), "cvf"], [("F", ob)])
            finals.append(dma("sp", [(yT_d[k * 128:(k + 1) * 128, tsl], Fb[ob][:, 0:512])], [], "out%d" % ob,
                              reads=[("F", ob)]))
    S.emit(nc, es, final_waits=finals)
    es.close()
    return nc


def prep_inputs(inp, NL=4):
    f = lambda a: np.ascontiguousarray(np.asarray(a, dtype=np.float32))
    sh = {}
    sh["w_in"] = f(inp["w_in"][:NL])
    sh["gla_w_out"] = f(inp["gla_w_out"][:NL])
    sh["sgu_w_out"] = f(inp["sgu_w_out"][:NL])
    sh["conv_w_out"] = f(inp["conv_w_out"][:NL])
    sh["w_o"] = f(inp["w_o"][:NL])
    sh["ffn_w_up"] = f(inp["ffn_w_up"][:NL])
    sh["ffn_w_down"] = f(inp["ffn_w_down"][:NL])
    sh["walpha17"] = f(np.concatenate([np.asarray(inp["gla_w_alpha"])[:NL], np.asarray(inp["gla_b_alpha"])[:NL, None, :]], axis=1))
    sh["wsT"] = f(np.transpose(np.asarray(inp["sgu_ws"])[:NL], (0, 3, 1, 2)).reshape(NL, 128, 512))
    cv = np.zeros((128, NL * CV_L + 8), np.float32)
    pm = lambda v, n: np.asarray(v, dtype=np.float32).reshape(n, 128).T
    for l in range(NL):
        b = l * CV_L
        cv[:, b:b + 8] = pm(inp["norm_mix"][l], 8)
        cv[:, b + 8:b + 16] = pm(inp["norm_ffn"][l], 8)
        cv[:, b + 16:b + 20] = pm(inp["gla_norm"][l], 4)
        for kk in range(3):
            cv[:, b + 20 + kk * 4:b + 24 + kk * 4] = pm(inp["conv_w"][l][kk], 4)
            cv[:, b + 32 + kk * 44:b + 32 + (kk + 1) * 44] = pm(inp["ffn_conv_w"][l][kk], 44)
        cv[:, b + 164:b + 208] = pm(inp["ffn_conv_b"][l], 44)
    cv[:, NL * CV_L:NL * CV_L + 8] = pm(inp["norm_final"], 8)
    sh["cvec"] = cv
    rb = np.zeros((NL, 128, 1536), np.float32)
    for l in range(NL):
        rb[l, :, 0:512] = np.asarray(inp["sgu_ln_g"])[l][None, :]
        rb[l, :, 512:1024] = np.asarray(inp["sgu_ln_b"])[l][None, :]
        rb[l, :, 1024:1536] = np.asarray(inp["sgu_bs"])[l].reshape(1, 512)
    sh["rowb"] = rb
    j = np.arange(128)[:, None]
    i = np.arange(128)[None, :]
    cm = np.zeros((128, 768), np.float32)
    cm[:, 0:128] = np.where(j <= i, -1.0 / 16.0, 0.0)
    cm[:, 128:256] = np.where(j > i, -1.0 / 16.0, 0.0)
    cm[:, 256:768] = np.tile(np.where(j <= i, 1.0, 0.0), (1, 4))
    sh["cmat"] = cm
    return sh


_CACHE = {}


def run(inputs, T, NL, ncores):
    key = (T, NL)
    if key not in _CACHE:
        _CACHE[key] = build(T, NL)
    nc = _CACHE[key]
    shared = prep_inputs(inputs, NL)
    x = np.asarray(inputs["x"], dtype=np.float32)
    in_maps = []
    for c in range(ncores):
        m = dict(shared)
        m["xT"] = np.ascontiguousarray(x[c, :T].T)
        in_maps.append(m)
    res = run_bass_kernel_spmd(nc, in_maps, core_ids=list(range(ncores)))
    out = np.stack([np.ascontiguousarray(r["yT"].T) for r in res.results], axis=0)
    return out.astype(np.float32)


def kernel(**inputs):
    return run(inputs, 2048, 4, 8)
```

```python
from contextlib import ExitStack
import numpy as np
import concourse.bass as bass
import concourse.mybir as mybir
from concourse.bass_utils import run_bass_kernel_spmd

F32 = mybir.dt.float32
BF16 = mybir.dt.bfloat16
AF = mybir.ActivationFunctionType
ALU = mybir.AluOpType

D = 1024
NK = 8
INC = 7184
DFF = 2816
NF = 22
EPS = 1e-6
CV_L = 208


class _Op:
    __slots__ = ("eng", "fn", "deps", "dma", "ndma", "signal", "count", "sem")


class Sched:
    ENGS = ("pe", "act", "dve", "pool", "sp")

    def __init__(self):
        self.ops = []
        self.by_eng = {e: [] for e in self.ENGS}
        self.last_w = {}
        self.readers = {}

    def add(self, eng, fn, reads=(), writes=(), dma=None, ndma=1):
        op = _Op()
        op.eng, op.fn, op.dma, op.ndma = eng, fn, dma, ndma
        op.signal = dma is not None
        op.count = 0
        op.sem = None
        deps = {}

        def need(d, raw):
            if d.dma is not None or dma is not None:
                return True
            if d.eng != eng:
                return True
            if eng == "pe":
                return False
            return True

        for r in reads:
            w = self.last_w.get(r)
            if w is not None and need(w, True):
                deps[id(w)] = w
        for r in writes:
            w = self.last_w.get(r)
            if w is not None and need(w, False):
                deps[id(w)] = w
            for rd in self.readers.get(r, {}).values():
                if rd is not op and need(rd, False):
                    deps[id(rd)] = rd
        op.deps = list(deps.values())
        for d in op.deps:
            d.signal = True
        for r in reads:
            self.readers.setdefault(r, {})[eng if dma is None else ("dma", len(self.ops))] = op
        for r in writes:
            self.last_w[r] = op
            self.readers[r] = {}
        self.ops.append(op)
        self.by_eng[eng].append(op)
        return op

    def emit(self, nc, es, final_waits=()):
        sems = {}

        def get_sem(key):
            if key not in sems:
                sems[key] = es.enter_context(nc.semaphore("s_%s_%s" % key))
            return sems[key]

        cnt = {}
        for op in self.ops:
            if op.dma is not None:
                key = ("d", op.dma)
                cnt[key] = cnt.get(key, 0) + 16 * op.ndma
                op.sem, op.count = key, cnt[key]
            elif op.signal:
                key = ("e", op.eng)
                cnt[key] = cnt.get(key, 0) + 1
                op.sem, op.count = key, cnt[key]
        for k in cnt:
            get_sem(k)
        block = es.enter_context(nc.Block())
        sections = {"pe": block.tensor, "act": block.scalar, "dve": block.vector,
                    "pool": block.gpsimd, "sp": block.sync}

        def make(engname):
            oplist = self.by_eng[engname]

            def body(e):
                waited = {}
                for op in oplist:
                    need = {}
                    for d in op.deps:
                        if need.get(d.sem, 0) < d.count:
                            need[d.sem] = d.count
                    for k, v in need.items():
                        if waited.get(k, 0) < v:
                            e.wait_ge(sems[k], v)
                            waited[k] = v
                    if op.dma is not None:
                        op.fn(e, sems[op.sem])
                    else:
                        ins = op.fn(e)
                        if op.signal:
                            ins.then_inc(sems[op.sem], 1)
                if engname == "sp":
                    for op in final_waits:
                        e.wait_ge(sems[op.sem], op.count)
            return body

        for en in self.ENGS:
            sections[en](make(en))


STAGE = 99
SUB = 99


def build(T=2048, NL=4):
    NST = T // 512
    GST = 256
    nc = bass.Bass("TRN2", target_bir_lowering=False)
    es = ExitStack()

    def dram(name, shape, kind="ExternalInput"):
        return nc.dram_tensor(name, list(shape), F32, kind=kind).ap()

    xT_d = dram("xT", [D, T])
    w_in_d = dram("w_in", [NL, D, INC])
    wA_d = [dram(n, [NL, 512, D]) for n in ("gla_w_out", "sgu_w_out", "conv_w_out")]
    w_o_d = dram("w_o", [NL, D, D])
    w_up_d = dram("ffn_w_up", [NL, D, 2 * DFF])
    w_dn_d = dram("ffn_w_down", [NL, DFF, D])
    walpha_d = dram("walpha17", [NL, 17, 256])
    wsT_d = dram("wsT", [NL, 128, 512])
    cvec_d = dram("cvec", [128, NL * CV_L + 8])
    rowb_d = dram("rowb", [NL, 128, 1536])
    cmat_d = dram("cmat", [128, 768])
    yT_d = dram("yT", [D, T], kind="ExternalOutput")

    def sb(name, shape, dt):
        return es.enter_context(nc.sbuf_tensor(name, list(shape), dt))

    xT = sb("xT_s", [128, NK, T], F32)
    hT = sb("hT_s", [128, NK, T], BF16)
    BR = sb("BR_s", [128, 4, T], BF16)
    WO = sb("WO_s", [128, NK, D], BF16)
    SLOT_E = 8320
    SLOT = [sb("slot%d" % i, [128, SLOT_E], BF16) for i in range(2)]
    SLW = sb("slotW", [128, 4, D], BF16)
    NFB, NHB = 6, 16
    Fb = [sb("F%d" % i, [128, 516], F32) for i in range(NFB)]
    Hb = [sb("H%d" % i, [128, 512], BF16) for i in range(NHB)]
    cvf = sb("cvf", [128, 8], F32)
    cvl = sb("cvl", [128, CV_L], F32)
    triU = sb("triU", [128, 128], BF16)
    triSU = sb("triSU", [128, 128], BF16)
    MASK = sb("MASK", [128, 512], BF16)
    ones_bf = sb("ones_bf", [128, 128], BF16)
    epsc = sb("epsc", [128, 1], F32)
    A17 = sb("A17", [32, GST], BF16)
    walpha = sb("walpha", [32, 256], BF16)
    WST = sb("WST", [128, 512], BF16)
    LNG = sb("LNG", [128, 512], BF16)
    LNB = sb("LNB", [128, 512], BF16)
    BSB = sb("BSB", [128, 512], F32)
    Sst = sb("Sst", [128, 256], F32)
    Sbf = sb("Sbf", [128, 256], BF16)
    DEC = sb("DEC", [128, 2], F32)
    STATS = [sb("STAT%d" % i, [128, 8], F32) for i in range(2)]
    MVS = [sb("MV%d" % i, [128, 4], F32) for i in range(2)]
    PS = [es.enter_context(nc.psum_tensor("ps%d" % i, [128, 512], F32)) for i in range(8)]

    S = Sched()
    bank_ctr = [0]

    def nb():
        b = bank_ctr[0] % 8
        bank_ctr[0] += 1
        return b

    def mm(bank, out_ap, pairs, reads):
        def fn(pe):
            n = len(pairs)
            ins = None
            for i, (l, r) in enumerate(pairs):
                ins = pe.matmul(out_ap, l, r, start=(i == 0), stop=(i == n - 1))
            return ins
        return S.add("pe", fn, reads=reads, writes=[("ps", bank)])

    def act(out, in_, func, reads, writes, bias=None, scale=None):
        kw = {}
        if bias is not None:
            kw["bias"] = bias
        if scale is not None:
            kw["scale"] = scale
        return S.add("act", lambda e: e.activation(out=out, in_=in_, func=func, **kw), reads=reads, writes=writes)

    def tt(out, in0, in1, op, reads, writes, eng="dve"):
        return S.add(eng, lambda e: e.tensor_tensor(out=out, in0=in0, in1=in1, op=op), reads=reads, writes=writes)

    def ts(out, in0, s1, s2, op0, op1, reads, writes, eng="dve"):
        if s2 is None:
            return S.add(eng, lambda e: e.tensor_scalar(out=out, in0=in0, scalar1=s1, scalar2=None, op0=op0),
                         reads=reads, writes=writes)
        return S.add(eng, lambda e: e.tensor_scalar(out=out, in0=in0, scalar1=s1, scalar2=s2, op0=op0, op1=op1),
                     reads=reads, writes=writes)

    def stt(out, in0, scalar, in1, op0, op1, reads, writes, eng="dve"):
        return S.add(eng, lambda e: e.scalar_tensor_tensor(out=out, in0=in0, scalar=scalar, in1=in1, op0=op0, op1=op1),
                     reads=reads, writes=writes)

    def cp(eng, out, in_, reads, writes):
        if eng == "act":
            return S.add(eng, lambda e: e.activation(out=out, in_=in_, func=AF.Copy), reads=reads, writes=writes)
        return S.add(eng, lambda e: e.tensor_copy(out=out, in_=in_), reads=reads, writes=writes)

    def ms(eng, ap, val, writes):
        return S.add(eng, lambda e: e.memset(ap, val), reads=[], writes=writes)

    def dma(eng, pairs, writes, semkey, reads=()):
        def fn(e, sem):
            for o, i in pairs:
                e.dma_start(out=o, in_=i).then_inc(sem, 16)
        return S.add(eng, fn, reads=reads, writes=writes, dma=semkey, ndma=len(pairs))

    def v3(ap, a):
        return ap.rearrange("p (a b) -> p a b", a=a)

    dma("sp", [(xT[:, k, :], xT_d[k * 128:(k + 1) * 128, :]) for k in range(NK)],
        [("xT", k, st) for k in range(NK) for st in range(NST)], "init")
    dma("sp", [(cvf[:, :], cvec_d[:, NL * CV_L:NL * CV_L + 8])], ["cvf"], "init2")
    dma("pool", [(MASK[:, :], cmat_d[:, 256:768]), (triU[:, :], cmat_d[:, 0:128]), (triSU[:, :], cmat_d[:, 128:256])],
        ["MASK", "tri"], "initp")
    ms("dve", ones_bf[:, :], 1.0, ["ones"])
    ms("dve", epsc[:, :], EPS, ["eps"])
    ms("dve", A17[:, :], 1.0, ["A17"])

    def rmsnorm_rstd(st, tsl):
        b = nb()
        sqs = []
        for k in range(NK):
            hb = Hb[8 + (k % 2)]
            hres = ("H", 8 + (k % 2))
            act(hb[:, :], xT[:, k, tsl], AF.Square, [("xT", k, st)], [hres])
            sqs.append((hb, hres))
            def fn(pe, k=k, hb=hb, b=b):
                return pe.matmul(PS[b][:, :], ones_bf[:, :], hb[:, :], start=(k == 0), stop=(k == NK - 1))
            S.add("pe", fn, reads=[hres, "ones"], writes=[("ps", b)])
        act(Fb[0][:, 0:512], PS[b][:, :], AF.Ln, [("ps", b), "eps"], [("F", 0)], bias=epsc[:, 0:1], scale=1.0 / D)
        act(Fb[0][:, 0:512], Fb[0][:, 0:512], AF.Exp, [("F", 0)], [("F", 0)], scale=-0.5)
        return 0

    def norm_to_h(l_col):
        for st in range(NST):
            tsl = slice(st * 512, (st + 1) * 512)
            rmsnorm_rstd(st, tsl)
            for k in range(NK):
                stt(hT[:, k, tsl], xT[:, k, tsl], cvl[:, l_col + k:l_col + k + 1], Fb[0][:, 0:512],
                    ALU.mult, ALU.mult, [("xT", k, st), ("F", 0), "cvl"], [("hT", st)])

    slot_ctr = [0]

    def next_slot():
        s = slot_ctr[0] % 2
        slot_ctr[0] += 1
        return s

    def load_group(s, pairs):
        dma("pool", pairs, [("slot", s)], "slot%d" % s)

    for l in range(NL):
        dma("sp", [(cvl[:, :], cvec_d[:, l * CV_L:(l + 1) * CV_L]),
                   (BSB[:, :], rowb_d[l, :, 1024:1536])], ["cvl", "BSB"], "lsmall")
        dma("pool", [(WST[:, :], wsT_d[l]), (LNG[:, :], rowb_d[l, :, 0:512]), (LNB[:, :], rowb_d[l, :, 512:1024]),
                     (walpha[0:17, :], walpha_d[l])],
            ["WST", "LNG", "LNB", "walpha"], "lsmallp")
        tt(WST[:, :], WST[:, :], MASK[:, :], ALU.mult, ["WST", "MASK"], ["WST"])
        dma("pool", [(WO[:, k, :], w_o_d[l, k * 128:(k + 1) * 128, :]) for k in range(NK)], ["WO"], "wo")

        if STAGE < 1:
            continue
        norm_to_h(0)
        if STAGE < 2:
            continue

        s = next_slot()
        Wg = SLOT[s][:, 0:NK * 512].rearrange("p (k c) -> p k c", k=NK)
        load_group(s, [(Wg[:, k, :], w_in_d[l, k * 128:(k + 1) * 128, 1024:1536]) for k in range(NK)])
        i = 0
        for st in range(NST):
            tsl = slice(st * 512, (st + 1) * 512)
            for h in range(4):
                b = nb()
                mm(b, PS[b][:, :], [(Wg[:, k, h * 128:(h + 1) * 128], hT[:, k, tsl]) for k in range(NK)],
                   [("slot", s), ("hT", st)])
                hb = 8 + (i % 2)
                i += 1
                act(Hb[hb][:, :], PS[b][:, :], AF.Sigmoid, [("ps", b)], [("H", hb)])
                stt(BR[:, h, tsl], PS[b][:, :], cvl[:, 16 + h:17 + h], Hb[hb][:, :], ALU.mult, ALU.mult,
                    [("ps", b), ("H", hb), "cvl"], [("BR", st)])

        if STAGE < 3:
            continue
        s = next_slot()
        Wm = SLOT[s][:, 0:NK * 1040].rearrange("p (k c) -> p k c", k=NK)
        prs = []
        for k in range(NK):
            prs.append((Wm[:, k, 0:1024], w_in_d[l, k * 128:(k + 1) * 128, 0:1024]))
            prs.append((Wm[:, k, 1024:1040], w_in_d[l, k * 128:(k + 1) * 128, 1536:1552]))
        load_group(s, prs)
        slr = ("slot", s)
        ms("dve", Sst[:, :], 0.0, ["S"])
        ms("dve", Sbf[:, :], 0.0, ["Sbf"])
        EB, ENB, QT, KT, Vb, KEb, STM, OSQ, TT_ = 0, 1, 2, 3, 4, 5, 6, 7, 8
        QTO = 10
        ms("dve", Hb[QT][:, :], 0.0, [("H", QT)])
        ms("dve", Hb[QTO][:, :], 0.0, [("H", QTO)])
        FELA, FR = 1, 2
        for gst in range(T // GST):
            g0 = gst * GST
            gsl = slice(g0, g0 + GST)
            st = g0 // 512
            hres = ("hT", st)
            b = nb()
            mm(b, PS[b][0:16, 0:GST], [(Wm[:, k, 1024:1040], hT[:, k, gsl]) for k in range(NK)], [slr, hres])
            if SUB < 1:
                continue
            act(A17[0:16, :], PS[b][0:16, 0:GST], AF.Copy, [("ps", b)], ["A17"])
            if SUB < 2:
                continue
            for t2 in range(GST // 128):
                loc = slice(t2 * 128, (t2 + 1) * 128)
                b = nb()
                mm(b, PS[b][:, 0:256], [(A17[0:17, loc], walpha[0:17, :])], ["A17", "walpha"])
                act(Fb[FELA][:, 0:256], PS[b][:, 0:256], AF.Exp, [("ps", b)], [("F", FELA)], scale=-1.0)
                act(Fb[FELA][:, 256:512], Fb[FELA][:, 0:256], AF.Ln, [("F", FELA)], [("F", FELA)], bias=1.0, scale=1.0)
                la = Fb[FELA]
                LAH, LAL = Hb[9][:, 0:256], Hb[9][:, 256:512]
                cp("dve", LAH, la[:, 256:512], [("F", FELA)], [("H", 9)])
                tt(LAL, la[:, 256:512], LAH, ALU.subtract, [("F", FELA), ("H", 9)], [("H", 9)])
                if SUB < 3:
                    continue
                b = nb()
                for cc in range(2):
                    mm(b, PS[b][:, cc * 128:(cc + 1) * 128],
                       [(LAH[:, cc * 128:(cc + 1) * 128], triU[:, :]), (LAL[:, cc * 128:(cc + 1) * 128], triU[:, :])],
                       [("H", 9), "tri"])
                mm(b, PS[b][:, 256:512], [(triSU[:, :], LAH), (triSU[:, :], LAL)], [("H", 9), "tri"])
                for cc in range(2):
                    act(Hb[EB][:, cc * GST + t2 * 128:cc * GST + (t2 + 1) * 128], PS[b][:, cc * 128:(cc + 1) * 128],
                        AF.Exp, [("ps", b)], [("H", EB)])
                    act(Hb[ENB][:, cc * GST + t2 * 128:cc * GST + (t2 + 1) * 128], PS[b][:, cc * 128:(cc + 1) * 128],
                        AF.Exp, [("ps", b)], [("H", ENB)], scale=-1.0)
                if SUB < 4:
                    continue
                act(Fb[3 + t2][:, 0:256], PS[b][:, 256:512], AF.Exp, [("ps", b)], [("F", 3 + t2)])
                act(Fb[3 + t2][:, 256:258], PS[b][:, 127:256:128], AF.Exp, [("ps", b)], [("F", 3 + t2)])
            if SUB < 5:
                continue
            for cc in range(2):
                b = nb()
                mm(b, PS[b][:, 0:GST], [(Wm[:, k, cc * 128:(cc + 1) * 128], hT[:, k, gsl]) for k in range(NK)], [slr, hres])
                stt(Hb[QT][0:64, cc * GST:(cc + 1) * GST], PS[b][0:64, 0:GST], 0.125, Hb[EB][0:64, cc * GST:(cc + 1) * GST],
                    ALU.mult, ALU.mult, [("ps", b), ("H", EB)], [("H", QT)])
                stt(Hb[QTO][64:128, cc * GST:(cc + 1) * GST], PS[b][64:128, 0:GST], 0.125, Hb[EB][64:128, cc * GST:(cc + 1) * GST],
                    ALU.mult, ALU.mult, [("ps", b), ("H", EB)], [("H", QTO)])
                b = nb()
                mm(b, PS[b][:, 0:GST], [(Wm[:, k, 256 + cc * 128:256 + (cc + 1) * 128], hT[:, k, gsl]) for k in range(NK)],
                   [slr, hres])
                tt(Hb[KT][:, cc * GST:(cc + 1) * GST], PS[b][:, 0:GST], Hb[ENB][:, cc * GST:(cc + 1) * GST], ALU.mult,
                   [("ps", b), ("H", ENB)], [("H", KT)])
            tstate = {}

            def gla_front(t2):
                t0 = g0 + t2 * 128
                tok = slice(t0, t0 + 128)
                fe = 3 + t2
                Vb, KEb, STM, OSQ, TT_ = ((4, 5, 6, 7, 8), (11, 12, 13, 14, 15))[t2]
                b = nb()
                mm(b, PS[b][:, :], [(hT[:, k, tok], Wm[:, k, 512:1024]) for k in range(NK)], [slr, hres])
                act(Hb[Vb][:, :], PS[b][:, :], AF.Copy, [("ps", b)], [("H", Vb)])
                b = nb()
                mm(b, PS[b][:, 0:256], [(hT[:, k, tok], Wm[:, k, 256:512]) for k in range(NK)], [slr, hres])
                tt(Hb[KEb][:, 0:256], PS[b][:, 0:256], Fb[fe][:, 0:256], ALU.mult, [("ps", b), ("F", fe)], [("H", KEb)])
                b = nb()
                for h in range(4):
                    cc = h // 2
                    c0 = cc * GST + t2 * 128
                    qz = QT if h % 2 == 0 else QTO
                    mm(b, PS[b][:, h * 128:(h + 1) * 128],
                       [(Hb[KT][:, c0:c0 + 128], Hb[qz][:, c0:c0 + 128])], [("H", KT), ("H", qz)])
                tt(Hb[STM][:, :], PS[b][:, :], MASK[:, :], ALU.mult, [("ps", b), "MASK"], [("H", STM)])

            def gla_mid(t2):
                fe = 3 + t2
                Vb, KEb, STM, OSQ, TT_ = ((4, 5, 6, 7, 8), (11, 12, 13, 14, 15))[t2]
                bo = nb()
                for h in range(4):
                    cc = h // 2
                    c0 = cc * GST + t2 * 128
                    qz = QT if h % 2 == 0 else QTO
                    mm(bo, PS[bo][:, h * 128:(h + 1) * 128],
                       [(Sbf[:, cc * 128:(cc + 1) * 128], Hb[qz][:, c0:c0 + 128]),
                        (Hb[Vb][:, h * 128:(h + 1) * 128], Hb[STM][:, h * 128:(h + 1) * 128])],
                       ["Sbf", ("H", qz), ("H", Vb), ("H", STM)])
                bc = nb()
                for cc in range(2):
                    mm(bc, PS[bc][:, cc * 256:(cc + 1) * 256],
                       [(Hb[KEb][:, cc * 128:(cc + 1) * 128], Hb[Vb][:, cc * 256:(cc + 1) * 256])], [("H", KEb), ("H", Vb)])
                for h in range(4):
                    cc, po = h // 2, (h % 2) * 64
                    hf = h % 2
                    stt(Sst[po:po + 64, cc * 128:(cc + 1) * 128], Sst[po:po + 64, cc * 128:(cc + 1) * 128],
                        Fb[fe][po:po + 64, 256 + cc:257 + cc],
                        PS[bc][po:po + 64, cc * 256 + hf * 128:cc * 256 + (hf + 1) * 128],
                        ALU.mult, ALU.add, ["S", ("F", fe), ("ps", bc)], ["S"])
                act(Sbf[:, :], Sst[:, :], AF.Copy, ["S"], ["Sbf"])
                act(Hb[OSQ][:, :], PS[bo][:, :], AF.Square, [("ps", bo)], [("H", OSQ)])
                tstate[t2] = bo

            def gla_tail(t2):
                t0 = g0 + t2 * 128
                tok = slice(t0, t0 + 128)
                Vb, KEb, STM, OSQ, TT_ = ((4, 5, 6, 7, 8), (11, 12, 13, 14, 15))[t2]
                FR = (2, 5)[t2]
                bo = tstate[t2]
                bs_ = nb()
                mm(bs_, PS[bs_][:, :], [(ones_bf[:, :], Hb[OSQ][:, :])], ["ones", ("H", OSQ)])
                act(Fb[FR][:, 0:512], PS[bs_][:, :], AF.Ln, [("ps", bs_), "eps"], [("F", FR)], bias=epsc[:, 0:1], scale=1.0 / 128)
                act(Fb[FR][:, 0:512], Fb[FR][:, 0:512], AF.Exp, [("F", FR)], [("F", FR)], scale=-0.5)
                tt(Hb[TT_][:, :], PS[bo][:, :], Fb[FR][:, 0:512], ALU.mult, [("ps", bo), ("F", FR)], [("H", TT_)])
                tt(BR[:, :, tok], v3(Hb[TT_][:, :], 4), BR[:, :, tok], ALU.mult, [("H", TT_), ("BR", st)], [("BR", st)])

            gla_front(0)
            gla_front(1)
            gla_mid(0)
            gla_mid(1)
            gla_tail(0)
            gla_tail(1)

        def out_stage(bi):
            s = next_slot()
            Wgt = SLOT[s][:, 0:NK * D].rearrange("p (k c) -> p k c", k=NK)
            c0 = 4112 + bi * 1024
            load_group(s, [(Wgt[:, k, :], w_in_d[l, k * 128:(k + 1) * 128, c0:c0 + 1024]) for k in range(NK)])
            dma("pool", [(SLW[:, kc, :], wA_d[bi][l, kc * 128:(kc + 1) * 128, :]) for kc in range(4)], ["SLW"], "slw")
            j = 0
            for st in range(NST):
                tsl = slice(st * 512, (st + 1) * 512)
                for dc in range(NK):
                    dsl = slice(dc * 128, (dc + 1) * 128)
                    by = nb()
                    mm(by, PS[by][:, :], [(SLW[:, kc, dsl], BR[:, kc, tsl]) for kc in range(4)], ["SLW", ("BR", st)])
                    bg = nb()
                    mm(bg, PS[bg][:, :], [(Wgt[:, k, dsl], hT[:, k, tsl]) for k in range(NK)], [("slot", s), ("hT", st)])
                    hb = 8 + (j % 2)
                    j += 1
                    act(Hb[hb][:, :], PS[bg][:, :], AF.Sigmoid, [("ps", bg)], [("H", hb)])
                    tt(Hb[dc][:, :], PS[by][:, :], Hb[hb][:, :], ALU.mult, [("ps", by), ("H", hb)], [("H", dc)])
                for dc in range(NK):
                    dsl = slice(dc * 128, (dc + 1) * 128)
                    b = nb()
                    mm(b, PS[b][:, :], [(WO[:, k, dsl], Hb[k][:, :]) for k in range(NK)], ["WO"] + [("H", k) for k in range(NK)])
                    tt(xT[:, dc, tsl], xT[:, dc, tsl], PS[b][:, :], ALU.add, [("xT", dc, st), ("ps", b)], [("xT", dc, st)])

        if STAGE < 4:
            continue
        out_stage(0)
        if STAGE < 5:
            continue

        s = next_slot()
        Ws = SLOT[s][:, 0:NK * 1024].rearrange("p (k c) -> p k c", k=NK)
        load_group(s, [(Ws[:, k, :], w_in_d[l, k * 128:(k + 1) * 128, 1552:2576]) for k in range(NK)])
        slr = ("slot", s)
        for st in range(NST):
            tsl = slice(st * 512, (st + 1) * 512)
            hres = ("hT", st)
            for c in range(4):
                b = nb()
                mm(b, PS[b][:, :], [(Ws[:, k, c * 128:(c + 1) * 128], hT[:, k, tsl]) for k in range(NK)], [slr, hres])
                act(Hb[c][:, :], PS[b][:, :], AF.Gelu_apprx_tanh, [("ps", b)], [("H", c)])
            def sgu_A(t4):
                t0 = st * 512 + t4 * 128
                tok = slice(t0, t0 + 128)
                p_ = t4 % 2
                f0_, f1_, h4_ = 3 * p_, 3 * p_ + 1, 4 + p_
                STAT, MV = STATS[p_], MVS[p_]
                b = nb()
                mm(b, PS[b][:, :], [(hT[:, k, tok], Ws[:, k, 512:1024]) for k in range(NK)], [slr, hres])
                act(Fb[f0_][:, 0:512], PS[b][:, :], AF.Gelu_apprx_tanh, [("ps", b)], [("F", f0_)])
                S.add("dve", lambda e, STAT=STAT, f0_=f0_: e.bn_stats(out=STAT[:, 0:6], in_=Fb[f0_][:, 0:512]),
                      reads=[("F", f0_)], writes=[("STAT", p_)])
                S.add("dve", lambda e, STAT=STAT, MV=MV: e.bn_aggr(out=MV[:, 0:2], in_=STAT[:, 0:6]),
                      reads=[("STAT", p_)], writes=[("MV", p_)])
                act(MV[:, 2:3], MV[:, 1:2], AF.Ln, [("MV", p_), "eps"], [("MV2", p_)], bias=epsc[:, 0:1], scale=1.0)
                act(MV[:, 3:4], MV[:, 2:3], AF.Exp, [("MV2", p_)], [("MV3", p_)], scale=-0.5)
                ts(Fb[f1_][:, 0:512], Fb[f0_][:, 0:512], MV[:, 0:1], MV[:, 3:4], ALU.subtract, ALU.mult,
                   [("F", f0_), ("MV", p_), ("MV3", p_)], [("F", f1_)])
                tt(Fb[f1_][:, 0:512], Fb[f1_][:, 0:512], LNG[:, :], ALU.mult, [("F", f1_), "LNG"], [("F", f1_)])
                tt(Hb[h4_][:, :], Fb[f1_][:, 0:512], LNB[:, :], ALU.add, [("F", f1_), "LNB"], [("H", h4_)])

            def sgu_B(t4):
                t0 = st * 512 + t4 * 128
                tok = slice(t0, t0 + 128)
                loc = slice(t4 * 128, (t4 + 1) * 128)
                p_ = t4 % 2
                f2_, h4_ = 3 * p_ + 2, 4 + p_
                bm = nb()
                for h in range(4):
                    hs = slice(h * 128, (h + 1) * 128)
                    mm(bm, PS[bm][:, hs], [(Hb[h4_][:, hs], WST[:, hs])], [("H", h4_), "WST"])
                tt(Fb[f2_][:, 0:512], PS[bm][:, :], BSB[:, :], ALU.add, [("ps", bm), "BSB"], [("F", f2_)])
                for h in range(4):
                    hs = slice(h * 128, (h + 1) * 128)
                    tt(BR[:, h, tok], Fb[f2_][:, hs], Hb[h][:, loc], ALU.mult, [("F", f2_), ("H", h)], [("BR", st)])

            sgu_A(0)
            sgu_A(1)
            sgu_B(0)
            sgu_A(2)
            sgu_B(1)
            sgu_A(3)
            sgu_B(2)
            sgu_B(3)
        if STAGE < 6:
            continue
        out_stage(1)
        if STAGE < 7:
            continue

        for cpair in range(2):
            s = next_slot()
            Wc = SLOT[s][:, 0:NK * 768].rearrange("p (k c) -> p k c", k=NK)
            prs = []
            for k in range(NK):
                for j3 in range(3):
                    c0 = 2576 + j3 * 512 + cpair * 256
                    prs.append((Wc[:, k, j3 * 256:(j3 + 1) * 256], w_in_d[l, k * 128:(k + 1) * 128, c0:c0 + 256]))
            load_group(s, prs)
            slr = ("slot", s)
            for ci in range(2):
                c = cpair * 2 + ci
                for st in range(NST):
                    tsl = slice(st * 512, (st + 1) * 512)
                    hres = ("hT", st)
                    pp = st % 2
                    bx, bc_, bb = nb(), nb(), nb()
                    mm(bx, PS[bx][:, :], [(Wc[:, k, 512 + ci * 128:512 + (ci + 1) * 128], hT[:, k, tsl]) for k in range(NK)], [slr, hres])
                    mm(bc_, PS[bc_][:, :], [(Wc[:, k, 256 + ci * 128:256 + (ci + 1) * 128], hT[:, k, tsl]) for k in range(NK)], [slr, hres])
                    mm(bb, PS[bb][:, :], [(Wc[:, k, ci * 128:(ci + 1) * 128], hT[:, k, tsl]) for k in range(NK)], [slr, hres])
                    act(Fb[2][:, 0:512], PS[bx][:, :], AF.Copy, [("ps", bx)], [("F", 2)])
                    if st == 0:
                        ms("dve", Fb[pp][:, 0:2], 0.0, [("F", pp)])
                    else:
                        cp("act", Fb[pp][:, 0:2], Fb[1 - pp][:, 512:514], [("F", 1 - pp)], [("F", pp)])
                    tt(Fb[pp][:, 2:514], PS[bc_][:, :], Fb[2][:, 0:512], ALU.mult, [("ps", bc_), ("F", 2)], [("F", pp)])
                    cw = lambda kk: cvl[:, 20 + kk * 4 + c:21 + kk * 4 + c]
                    ts(Fb[3][:, 0:512], Fb[pp][:, 2:514], cw(2), None, ALU.mult, None, [("F", pp), "cvl"], [("F", 3)])
                    stt(Fb[3][:, 0:512], Fb[pp][:, 1:513], cw(1), Fb[3][:, 0:512], ALU.mult, ALU.add, [("F", pp), ("F", 3), "cvl"], [("F", 3)])
                    stt(Fb[3][:, 0:512], Fb[pp][:, 0:512], cw(0), Fb[3][:, 0:512], ALU.mult, ALU.add, [("F", pp), ("F", 3), "cvl"], [("F", 3)])
                    tt(BR[:, c, tsl], Fb[3][:, 0:512], PS[bb][:, :], ALU.mult, [("F", 3), ("ps", bb)], [("BR", st)])
        if STAGE < 8:
            continue
        out_stage(2)
        if STAGE < 9:
            continue

        norm_to_h(8)
        f0 = 0
        while f0 < NF:
            G = min(4, NF - f0)
            s = next_slot()
            Wu = SLOT[s][:, 0:NK * 1024].rearrange("p (k c) -> p k c", k=NK)
            prs = []
            for k in range(NK):
                prs.append((Wu[:, k, 0:G * 128], w_up_d[l, k * 128:(k + 1) * 128, f0 * 128:(f0 + G) * 128]))
                prs.append((Wu[:, k, 512:512 + G * 128], w_up_d[l, k * 128:(k + 1) * 128, DFF + f0 * 128:DFF + (f0 + G) * 128]))
            load_group(s, prs)
            dma("pool", [(SLW[:, j, :], w_dn_d[l, (f0 + j) * 128:(f0 + j + 1) * 128, :]) for j in range(G)], ["SLW"], "slw")
            slr = ("slot", s)
            for j in range(G):
                f = f0 + j
                cg = lambda kk: cvl[:, 32 + kk * 44 + f:33 + kk * 44 + f]
                cvv = lambda kk: cvl[:, 32 + kk * 44 + 22 + f:33 + kk * 44 + 22 + f]
                bgc = cvl[:, 164 + f:165 + f]
                bvc = cvl[:, 164 + 22 + f:165 + 22 + f]
                for st in range(NST):
                    tsl = slice(st * 512, (st + 1) * 512)
                    hres = ("hT", st)
                    pp = st % 2
                    ug, uv = pp, 2 + pp
                    bg, bv = nb(), nb()
                    mm(bg, PS[bg][:, :], [(Wu[:, k, j * 128:(j + 1) * 128], hT[:, k, tsl]) for k in range(NK)], [slr, hres])
                    mm(bv, PS[bv][:, :], [(Wu[:, k, 512 + j * 128:512 + (j + 1) * 128], hT[:, k, tsl]) for k in range(NK)], [slr, hres])
                    chains = ((ug, bg, 4, cg, bgc), (uv, bv, 5, cvv, bvc))
                    for (u, b_, dst, cw_, bc_) in chains:
                        if st == 0:
                            ms("dve", Fb[u][:, 0:2], 0.0, [("F", u)])
                        else:
                            uo = u - pp + (1 - pp)
                            cp("act", Fb[u][:, 0:2], Fb[uo][:, 512:514], [("F", uo)], [("F", u)])
                        act(Fb[u][:, 2:514], PS[b_][:, :], AF.Copy, [("ps", b_)], [("F", u)])
                    for (u, b_, dst, cw_, bc_) in chains:
                        act(Fb[dst][:, 0:512], PS[b_][:, :], AF.Identity, [("ps", b_), "cvl"], [("F", dst)],
                            bias=bc_, scale=cw_(2))
                    for (u, b_, dst, cw_, bc_) in chains:
                        stt(Fb[dst][:, 0:512], Fb[u][:, 1:513], cw_(1), Fb[dst][:, 0:512], ALU.mult, ALU.add,
                            [("F", u), ("F", dst), "cvl"], [("F", dst)])
                        stt(Fb[dst][:, 0:512], Fb[u][:, 0:512], cw_(0), Fb[dst][:, 0:512], ALU.mult, ALU.add,
                            [("F", u), ("F", dst), "cvl"], [("F", dst)])
                    hb = 8 + ((j * NST + st) % 2)
                    act(Hb[hb][:, :], Fb[4][:, 0:512], AF.Silu, [("F", 4)], [("H", hb)])
                    tt(BR[:, j, tsl], Hb[hb][:, :], Fb[5][:, 0:512], ALU.mult, [("H", hb), ("F", 5)], [("BR", st)])
            for st in range(NST):
                tsl = slice(st * 512, (st + 1) * 512)
                for dc in range(NK):
                    dsl = slice(dc * 128, (dc + 1) * 128)
                    b = nb()
                    mm(b, PS[b][:, :], [(SLW[:, j, dsl], BR[:, j, tsl]) for j in range(G)], ["SLW", ("BR", st)])
                    tt(xT[:, dc, tsl], xT[:, dc, tsl], PS[b][:, :], ALU.add, [("xT", dc, st), ("ps", b)], [("xT", dc, st)])
            f0 += G

    finals = []
    for st in range(NST):
        tsl = slice(st * 512, (st + 1) * 512)
        rmsnorm_rstd(st, tsl)
        for k in range(NK):
            ob = 1 + (k % 4)
            stt(Fb[ob][:, 0:512], xT[:, k, tsl], cvf[:, k:k + 1], Fb[0][:, 0:512], ALU.mult, ALU.mult,
                [("xT", k, st), ("F", 0), "cvf"], [("F", ob)])
            finals.append(dma("sp", [(yT_d[k * 128:(k + 1) * 128, tsl], Fb[ob][:, 0:512])], [], "out%d" % ob,
                              reads=[("F", ob)]))
    S.emit(nc, es, final_waits=finals)
    es.close()
    return nc


def prep_inputs(inp, NL=4):
    f = lambda a: np.ascontiguousarray(np.asarray(a, dtype=np.float32))
    sh = {}
    sh["w_in"] = f(inp["w_in"][:NL])
    sh["gla_w_out"] = f(inp["gla_w_out"][:NL])
    sh["sgu_w_out"] = f(inp["sgu_w_out"][:NL])
    sh["conv_w_out"] = f(inp["conv_w_out"][:NL])
    sh["w_o"] = f(inp["w_o"][:NL])
    sh["ffn_w_up"] = f(inp["ffn_w_up"][:NL])
    sh["ffn_w_down"] = f(inp["ffn_w_down"][:NL])
    sh["walpha17"] = f(np.concatenate([np.asarray(inp["gla_w_alpha"])[:NL], np.asarray(inp["gla_b_alpha"])[:NL, None, :]], axis=1))
    sh["wsT"] = f(np.transpose(np.asarray(inp["sgu_ws"])[:NL], (0, 3, 1, 2)).reshape(NL, 128, 512))
    cv = np.zeros((128, NL * CV_L + 8), np.float32)
    pm = lambda v, n: np.asarray(v, dtype=np.float32).reshape(n, 128).T
    for l in range(NL):
        b = l * CV_L
        cv[:, b:b + 8] = pm(inp["norm_mix"][l], 8)
        cv[:, b + 8:b + 16] = pm(inp["norm_ffn"][l], 8)
        cv[:, b + 16:b + 20] = pm(inp["gla_norm"][l], 4)
        for kk in range(3):
            cv[:, b + 20 + kk * 4:b + 24 + kk * 4] = pm(inp["conv_w"][l][kk], 4)
            cv[:, b + 32 + kk * 44:b + 32 + (kk + 1) * 44] = pm(inp["ffn_conv_w"][l][kk], 44)
        cv[:, b + 164:b + 208] = pm(inp["ffn_conv_b"][l], 44)
    cv[:, NL * CV_L:NL * CV_L + 8] = pm(inp["norm_final"], 8)
    sh["cvec"] = cv
    rb = np.zeros((NL, 128, 1536), np.float32)
    for l in range(NL):
        rb[l, :, 0:512] = np.asarray(inp["sgu_ln_g"])[l][None, :]
        rb[l, :, 512:1024] = np.asarray(inp["sgu_ln_b"])[l][None, :]
        rb[l, :, 1024:1536] = np.asarray(inp["sgu_bs"])[l].reshape(1, 512)
    sh["rowb"] = rb
    j = np.arange(128)[:, None]
    i = np.arange(128)[None, :]
    cm = np.zeros((128, 768), np.float32)
    cm[:, 0:128] = np.where(j <= i, -1.0 / 16.0, 0.0)
    cm[:, 128:256] = np.where(j > i, -1.0 / 16.0, 0.0)
    cm[:, 256:768] = np.tile(np.where(j <= i, 1.0, 0.0), (1, 4))
    sh["cmat"] = cm
    return sh


_CACHE = {}


def run(inputs, T, NL, ncores):
    key = (T, NL)
    if key not in _CACHE:
        _CACHE[key] = build(T, NL)
    nc = _CACHE[key]
    shared = prep_inputs(inputs, NL)
    x = np.asarray(inputs["x"], dtype=np.float32)
    in_maps = []
    for c in range(ncores):
        m = dict(shared)
        m["xT"] = np.ascontiguousarray(x[c, :T].T)
        in_maps.append(m)
    res = run_bass_kernel_spmd(nc, in_maps, core_ids=list(range(ncores)))
    out = np.stack([np.ascontiguousarray(r["yT"].T) for r in res.results], axis=0)
    return out.astype(np.float32)


def kernel(**inputs):
    return run(inputs, 2048, 4, 8)
```

```python
from contextlib import ExitStack
import numpy as np
import concourse.bass as bass
import concourse.mybir as mybir
from concourse.bass_utils import run_bass_kernel_spmd

F32 = mybir.dt.float32
BF16 = mybir.dt.bfloat16
AF = mybir.ActivationFunctionType
ALU = mybir.AluOpType

D = 1024
NK = 8
INC = 7184
DFF = 2816
NF = 22
EPS = 1e-6
CV_L = 208


class _Op:
    __slots__ = ("eng", "fn", "deps", "dma", "ndma", "signal", "count", "sem")


class Sched:
    ENGS = ("pe", "act", "dve", "pool", "sp")

    def __init__(self):
        self.ops = []
        self.by_eng = {e: [] for e in self.ENGS}
        self.last_w = {}
        self.readers = {}

    def add(self, eng, fn, reads=(), writes=(), dma=None, ndma=1):
        op = _Op()
        op.eng, op.fn, op.dma, op.ndma = eng, fn, dma, ndma
        op.signal = dma is not None
        op.count = 0
        op.sem = None
        deps = {}

        def need(d, raw):
            if d.dma is not None or dma is not None:
                return True
            if d.eng != eng:
                return True
            if eng == "pe":
                return False
            return True

        for r in reads:
            w = self.last_w.get(r)
            if w is not None and need(w, True):
                deps[id(w)] = w
        for r in writes:
            w = self.last_w.get(r)
            if w is not None and need(w, False):
                deps[id(w)] = w
            for rd in self.readers.get(r, {}).values():
                if rd is not op and need(rd, False):
                    deps[id(rd)] = rd
        op.deps = list(deps.values())
        for d in op.deps:
            d.signal = True
        for r in reads:
            self.readers.setdefault(r, {})[eng if dma is None else ("dma", len(self.ops))] = op
        for r in writes:
            self.last_w[r] = op
            self.readers[r] = {}
        self.ops.append(op)
        self.by_eng[eng].append(op)
        return op

    def emit(self, nc, es, final_waits=()):
        sems = {}

        def get_sem(key):
            if key not in sems:
                sems[key] = es.enter_context(nc.semaphore("s_%s_%s" % key))
            return sems[key]

        cnt = {}
        for op in self.ops:
            if op.dma is not None:
                key = ("d", op.dma)
                cnt[key] = cnt.get(key, 0) + 16 * op.ndma
                op.sem, op.count = key, cnt[key]
            elif op.signal:
                key = ("e", op.eng)
                cnt[key] = cnt.get(key, 0) + 1
                op.sem, op.count = key, cnt[key]
        for k in cnt:
            get_sem(k)
        block = es.enter_context(nc.Block())
        sections = {"pe": block.tensor, "act": block.scalar, "dve": block.vector,
                    "pool": block.gpsimd, "sp": block.sync}

        def make(engname):
            oplist = self.by_eng[engname]

            def body(e):
                waited = {}
                for op in oplist:
                    need = {}
                    for d in op.deps:
                        if need.get(d.sem, 0) < d.count:
                            need[d.sem] = d.count
                    for k, v in need.items():
                        if waited.get(k, 0) < v:
                            e.wait_ge(sems[k], v)
                            waited[k] = v
                    if op.dma is not None:
                        op.fn(e, sems[op.sem])
                    else:
                        ins = op.fn(e)
                        if op.signal:
                            ins.then_inc(sems[op.sem], 1)
                if engname == "sp":
                    for op in final_waits:
                        e.wait_ge(sems[op.sem], op.count)
            return body

        for en in self.ENGS:
            sections[en](make(en))


STAGE = 99
SUB = 99


def build(T=2048, NL=4):
    NST = T // 512
    GST = 256
    nc = bass.Bass("TRN2", target_bir_lowering=False)
    es = ExitStack()

    def dram(name, shape, kind="ExternalInput"):
        return nc.dram_tensor(name, list(shape), F32, kind=kind).ap()

    xT_d = dram("xT", [D, T])
    w_in_d = dram("w_in", [NL, D, INC])
    wA_d = [dram(n, [NL, 512, D]) for n in ("gla_w_out", "sgu_w_out", "conv_w_out")]
    w_o_d = dram("w_o", [NL, D, D])
    w_up_d = dram("ffn_w_up", [NL, D, 2 * DFF])
    w_dn_d = dram("ffn_w_down", [NL, DFF, D])
    walpha_d = dram("walpha17", [NL, 17, 256])
    wsT_d = dram("wsT", [NL, 128, 512])
    cvec_d = dram("cvec", [128, NL * CV_L + 8])
    rowb_d = dram("rowb", [NL, 128, 1536])
    cmat_d = dram("cmat", [128, 768])
    yT_d = dram("yT", [D, T], kind="ExternalOutput")

    def sb(name, shape, dt):
        return es.enter_context(nc.sbuf_tensor(name, list(shape), dt))

    xT = sb("xT_s", [128, NK, T], F32)
    hT = sb("hT_s", [128, NK, T], BF16)
    BR = sb("BR_s", [128, 4, T], BF16)
    WO = sb("WO_s", [128, NK, D], BF16)
    SLOT_E = 8320
    SLOT = [sb("slot%d" % i, [128, SLOT_E], BF16) for i in range(2)]
    SLW = sb("slotW", [128, 4, D], BF16)
    NFB, NHB = 6, 16
    Fb = [sb("F%d" % i, [128, 516], F32) for i in range(NFB)]
    HP = [sb("HP%d" % i, [128, 1024], BF16) for i in range(NHB // 2)]
    Hb = [HP[i // 2][:, (i % 2) * 512:(i % 2 + 1) * 512] for i in range(NHB)]
    FX = [HP[i][:, :].bitcast(F32) for i in range(NHB // 2)]
    cvf = sb("cvf", [128, 8], F32)
    cvl = sb("cvl", [128, CV_L], F32)
    triU = sb("triU", [128, 128], BF16)
    triSU = sb("triSU", [128, 128], BF16)
    MASK = sb("MASK", [128, 512], BF16)
    ones_bf = sb("ones_bf", [128, 128], BF16)
    epsc = sb("epsc", [128, 1], F32)
    A17 = sb("A17", [32, GST], BF16)
    walpha = sb("walpha", [32, 256], BF16)
    WST = sb("WST", [128, 512], BF16)
    LNG = sb("LNG", [128, 512], BF16)
    LNB = sb("LNB", [128, 512], BF16)
    BSB = sb("BSB", [128, 512], F32)
    Sst = sb("Sst", [128, 256], F32)
    Sbf = sb("Sbf", [128, 256], BF16)
    DEC = sb("DEC", [128, 2], F32)
    STATS = [sb("STAT%d" % i, [128, 8], F32) for i in range(2)]
    MVS = [sb("MV%d" % i, [128, 4], F32) for i in range(2)]
    PS = [es.enter_context(nc.psum_tensor("ps%d" % i, [128, 512], F32)) for i in range(8)]

    S = Sched()
    bank_ctr = [0]

    def nb():
        b = bank_ctr[0] % 8
        bank_ctr[0] += 1
        return b

    def mm(bank, out_ap, pairs, reads):
        def fn(pe):
            n = len(pairs)
            ins = None
            for i, (l, r) in enumerate(pairs):
                ins = pe.matmul(out_ap, l, r, start=(i == 0), stop=(i == n - 1))
            return ins
        return S.add("pe", fn, reads=reads, writes=[("ps", bank)])

    def act(out, in_, func, reads, writes, bias=None, scale=None):
        kw = {}
        if bias is not None:
            kw["bias"] = bias
        if scale is not None:
            kw["scale"] = scale
        return S.add("act", lambda e: e.activation(out=out, in_=in_, func=func, **kw), reads=reads, writes=writes)

    def tt(out, in0, in1, op, reads, writes, eng="dve"):
        return S.add(eng, lambda e: e.tensor_tensor(out=out, in0=in0, in1=in1, op=op), reads=reads, writes=writes)

    def ts(out, in0, s1, s2, op0, op1, reads, writes, eng="dve"):
        if s2 is None:
            return S.add(eng, lambda e: e.tensor_scalar(out=out, in0=in0, scalar1=s1, scalar2=None, op0=op0),
                         reads=reads, writes=writes)
        return S.add(eng, lambda e: e.tensor_scalar(out=out, in0=in0, scalar1=s1, scalar2=s2, op0=op0, op1=op1),
                     reads=reads, writes=writes)

    def stt(out, in0, scalar, in1, op0, op1, reads, writes, eng="dve"):
        return S.add(eng, lambda e: e.scalar_tensor_tensor(out=out, in0=in0, scalar=scalar, in1=in1, op0=op0, op1=op1),
                     reads=reads, writes=writes)

    def cp(eng, out, in_, reads, writes):
        if eng == "act":
            return S.add(eng, lambda e: e.activation(out=out, in_=in_, func=AF.Copy), reads=reads, writes=writes)
        return S.add(eng, lambda e: e.tensor_copy(out=out, in_=in_), reads=reads, writes=writes)

    def ms(eng, ap, val, writes):
        return S.add(eng, lambda e: e.memset(ap, val), reads=[], writes=writes)

    def dma(eng, pairs, writes, semkey, reads=()):
        def fn(e, sem):
            for o, i in pairs:
                e.dma_start(out=o, in_=i).then_inc(sem, 16)
        return S.add(eng, fn, reads=reads, writes=writes, dma=semkey, ndma=len(pairs))

    def v3(ap, a):
        return ap.rearrange("p (a b) -> p a b", a=a)

    dma("sp", [(xT[:, k, :], xT_d[k * 128:(k + 1) * 128, :]) for k in range(NK)],
        [("xT", k, st) for k in range(NK) for st in range(NST)], "init")
    dma("sp", [(cvf[:, :], cvec_d[:, NL * CV_L:NL * CV_L + 8])], ["cvf"], "init2")
    dma("pool", [(MASK[:, :], cmat_d[:, 256:768]), (triU[:, :], cmat_d[:, 0:128]), (triSU[:, :], cmat_d[:, 128:256])],
        ["MASK", "tri"], "initp")
    ms("dve", ones_bf[:, :], 1.0, ["ones"])
    ms("dve", epsc[:, :], EPS, ["eps"])
    ms("dve", A17[:, :], 1.0, ["A17"])

    def rmsnorm_rstd(st, tsl):
        b = nb()
        sqs = []
        for k in range(NK):
            hb = Hb[8 + (k % 2)]
            hres = ("H", 8 + (k % 2))
            act(hb[:, :], xT[:, k, tsl], AF.Square, [("xT", k, st)], [hres])
            sqs.append((hb, hres))
            def fn(pe, k=k, hb=hb, b=b):
                return pe.matmul(PS[b][:, :], ones_bf[:, :], hb[:, :], start=(k == 0), stop=(k == NK - 1))
            S.add("pe", fn, reads=[hres, "ones"], writes=[("ps", b)])
        act(Fb[0][:, 0:512], PS[b][:, :], AF.Ln, [("ps", b), "eps"], [("F", 0)], bias=epsc[:, 0:1], scale=1.0 / D)
        act(Fb[0][:, 0:512], Fb[0][:, 0:512], AF.Exp, [("F", 0)], [("F", 0)], scale=-0.5)
        return 0

    def norm_to_h(l_col):
        for st in range(NST):
            tsl = slice(st * 512, (st + 1) * 512)
            rmsnorm_rstd(st, tsl)
            for k in range(NK):
                stt(hT[:, k, tsl], xT[:, k, tsl], cvl[:, l_col + k:l_col + k + 1], Fb[0][:, 0:512],
                    ALU.mult, ALU.mult, [("xT", k, st), ("F", 0), "cvl"], [("hT", st)])

    slot_ctr = [0]

    def next_slot():
        s = slot_ctr[0] % 2
        slot_ctr[0] += 1
        return s

    def load_group(s, pairs):
        dma("pool", pairs, [("slot", s)], "slot%d" % s)

    for l in range(NL):
        dma("sp", [(cvl[:, :], cvec_d[:, l * CV_L:(l + 1) * CV_L]),
                   (BSB[:, :], rowb_d[l, :, 1024:1536])], ["cvl", "BSB"], "lsmall")
        dma("pool", [(WST[:, :], wsT_d[l]), (LNG[:, :], rowb_d[l, :, 0:512]), (LNB[:, :], rowb_d[l, :, 512:1024]),
                     (walpha[0:17, :], walpha_d[l])],
            ["WST", "LNG", "LNB", "walpha"], "lsmallp")
        tt(WST[:, :], WST[:, :], MASK[:, :], ALU.mult, ["WST", "MASK"], ["WST"])
        dma("pool", [(WO[:, k, :], w_o_d[l, k * 128:(k + 1) * 128, :]) for k in range(NK)], ["WO"], "wo")

        if STAGE < 1:
            continue
        norm_to_h(0)
        if STAGE < 2:
            continue

        s = next_slot()
        Wg = SLOT[s][:, 0:NK * 512].rearrange("p (k c) -> p k c", k=NK)
        load_group(s, [(Wg[:, k, :], w_in_d[l, k * 128:(k + 1) * 128, 1024:1536]) for k in range(NK)])
        i = 0
        for st in range(NST):
            tsl = slice(st * 512, (st + 1) * 512)
            for h in range(4):
                b = nb()
                mm(b, PS[b][:, :], [(Wg[:, k, h * 128:(h + 1) * 128], hT[:, k, tsl]) for k in range(NK)],
                   [("slot", s), ("hT", st)])
                hb = 8 + (i % 2)
                i += 1
                act(Hb[hb][:, :], PS[b][:, :], AF.Sigmoid, [("ps", b)], [("H", hb)])
                stt(BR[:, h, tsl], PS[b][:, :], cvl[:, 16 + h:17 + h], Hb[hb][:, :], ALU.mult, ALU.mult,
                    [("ps", b), ("H", hb), "cvl"], [("BR", st)])

        if STAGE < 3:
            continue
        s = next_slot()
        Wm = SLOT[s][:, 0:NK * 1040].rearrange("p (k c) -> p k c", k=NK)
        prs = []
        for k in range(NK):
            prs.append((Wm[:, k, 0:1024], w_in_d[l, k * 128:(k + 1) * 128, 0:1024]))
            prs.append((Wm[:, k, 1024:1040], w_in_d[l, k * 128:(k + 1) * 128, 1536:1552]))
        load_group(s, prs)
        slr = ("slot", s)
        ms("dve", Sst[:, :], 0.0, ["S"])
        ms("dve", Sbf[:, :], 0.0, ["Sbf"])
        EB, ENB, QT, KT, Vb, KEb, STM, OSQ, TT_ = 0, 1, 2, 3, 4, 5, 6, 7, 8
        QTO = 10
        ms("dve", Hb[QT][:, :], 0.0, [("H", QT)])
        ms("dve", Hb[QTO][:, :], 0.0, [("H", QTO)])
        FELA, FR = 1, 2
        for gst in range(T // GST):
            g0 = gst * GST
            gsl = slice(g0, g0 + GST)
            st = g0 // 512
            hres = ("hT", st)
            b = nb()
            mm(b, PS[b][0:16, 0:GST], [(Wm[:, k, 1024:1040], hT[:, k, gsl]) for k in range(NK)], [slr, hres])
            if SUB < 1:
                continue
            act(A17[0:16, :], PS[b][0:16, 0:GST], AF.Copy, [("ps", b)], ["A17"])
            if SUB < 2:
                continue
            for t2 in range(GST // 128):
                loc = slice(t2 * 128, (t2 + 1) * 128)
                b = nb()
                mm(b, PS[b][:, 0:256], [(A17[0:17, loc], walpha[0:17, :])], ["A17", "walpha"])
                act(Fb[FELA][:, 0:256], PS[b][:, 0:256], AF.Exp, [("ps", b)], [("F", FELA)], scale=-1.0)
                act(Fb[FELA][:, 256:512], Fb[FELA][:, 0:256], AF.Ln, [("F", FELA)], [("F", FELA)], bias=1.0, scale=1.0)
                la = Fb[FELA]
                LAH, LAL = Hb[9][:, 0:256], Hb[9][:, 256:512]
                cp("dve", LAH, la[:, 256:512], [("F", FELA)], [("H", 9)])
                tt(LAL, la[:, 256:512], LAH, ALU.subtract, [("F", FELA), ("H", 9)], [("H", 9)])
                if SUB < 3:
                    continue
                b = nb()
                for cc in range(2):
                    mm(b, PS[b][:, cc * 128:(cc + 1) * 128],
                       [(LAH[:, cc * 128:(cc + 1) * 128], triU[:, :]), (LAL[:, cc * 128:(cc + 1) * 128], triU[:, :])],
                       [("H", 9), "tri"])
                mm(b, PS[b][:, 256:512], [(triSU[:, :], LAH), (triSU[:, :], LAL)], [("H", 9), "tri"])
                for cc in range(2):
                    act(Hb[EB][:, cc * GST + t2 * 128:cc * GST + (t2 + 1) * 128], PS[b][:, cc * 128:(cc + 1) * 128],
                        AF.Exp, [("ps", b)], [("H", EB)])
                    act(Hb[ENB][:, cc * GST + t2 * 128:cc * GST + (t2 + 1) * 128], PS[b][:, cc * 128:(cc + 1) * 128],
                        AF.Exp, [("ps", b)], [("H", ENB)], scale=-1.0)
                if SUB < 4:
                    continue
                act(Fb[3 + t2][:, 0:256], PS[b][:, 256:512], AF.Exp, [("ps", b)], [("F", 3 + t2)])
                act(Fb[3 + t2][:, 256:258], PS[b][:, 127:256:128], AF.Exp, [("ps", b)], [("F", 3 + t2)])
            if SUB < 5:
                continue
            for cc in range(2):
                b = nb()
                mm(b, PS[b][:, 0:GST], [(Wm[:, k, cc * 128:(cc + 1) * 128], hT[:, k, gsl]) for k in range(NK)], [slr, hres])
                stt(Hb[QT][0:64, cc * GST:(cc + 1) * GST], PS[b][0:64, 0:GST], 0.125, Hb[EB][0:64, cc * GST:(cc + 1) * GST],
                    ALU.mult, ALU.mult, [("ps", b), ("H", EB)], [("H", QT)])
                stt(Hb[QTO][64:128, cc * GST:(cc + 1) * GST], PS[b][64:128, 0:GST], 0.125, Hb[EB][64:128, cc * GST:(cc + 1) * GST],
                    ALU.mult, ALU.mult, [("ps", b), ("H", EB)], [("H", QTO)])
                b = nb()
                mm(b, PS[b][:, 0:GST], [(Wm[:, k, 256 + cc * 128:256 + (cc + 1) * 128], hT[:, k, gsl]) for k in range(NK)],
                   [slr, hres])
                tt(Hb[KT][:, cc * GST:(cc + 1) * GST], PS[b][:, 0:GST], Hb[ENB][:, cc * GST:(cc + 1) * GST], ALU.mult,
                   [("ps", b), ("H", ENB)], [("H", KT)])
            tstate = {}

            def gla_front(t2):
                t0 = g0 + t2 * 128
                tok = slice(t0, t0 + 128)
                fe = 3 + t2
                Vb, KEb, STM, OSQ, TT_ = ((4, 5, 6, 7, 8), (11, 12, 13, 14, 15))[t2]
                b = nb()
                mm(b, PS[b][:, :], [(hT[:, k, tok], Wm[:, k, 512:1024]) for k in range(NK)], [slr, hres])
                act(Hb[Vb][:, :], PS[b][:, :], AF.Copy, [("ps", b)], [("H", Vb)])
                b = nb()
                mm(b, PS[b][:, 0:256], [(hT[:, k, tok], Wm[:, k, 256:512]) for k in range(NK)], [slr, hres])
                tt(Hb[KEb][:, 0:256], PS[b][:, 0:256], Fb[fe][:, 0:256], ALU.mult, [("ps", b), ("F", fe)], [("H", KEb)])
                b = nb()
                for h in range(4):
                    cc = h // 2
                    c0 = cc * GST + t2 * 128
                    qz = QT if h % 2 == 0 else QTO
                    mm(b, PS[b][:, h * 128:(h + 1) * 128],
                       [(Hb[KT][:, c0:c0 + 128], Hb[qz][:, c0:c0 + 128])], [("H", KT), ("H", qz)])
                tt(Hb[STM][:, :], PS[b][:, :], MASK[:, :], ALU.mult, [("ps", b), "MASK"], [("H", STM)])

            def gla_mid(t2):
                fe = 3 + t2
                Vb, KEb, STM, OSQ, TT_ = ((4, 5, 6, 7, 8), (11, 12, 13, 14, 15))[t2]
                bo = nb()
                for h in range(4):
                    cc = h // 2
                    c0 = cc * GST + t2 * 128
                    qz = QT if h % 2 == 0 else QTO
                    mm(bo, PS[bo][:, h * 128:(h + 1) * 128],
                       [(Sbf[:, cc * 128:(cc + 1) * 128], Hb[qz][:, c0:c0 + 128]),
                        (Hb[Vb][:, h * 128:(h + 1) * 128], Hb[STM][:, h * 128:(h + 1) * 128])],
                       ["Sbf", ("H", qz), ("H", Vb), ("H", STM)])
                bc = nb()
                for cc in range(2):
                    mm(bc, PS[bc][:, cc * 256:(cc + 1) * 256],
                       [(Hb[KEb][:, cc * 128:(cc + 1) * 128], Hb[Vb][:, cc * 256:(cc + 1) * 256])], [("H", KEb), ("H", Vb)])
                for h in range(4):
                    cc, po = h // 2, (h % 2) * 64
                    hf = h % 2
                    stt(Sst[po:po + 64, cc * 128:(cc + 1) * 128], Sst[po:po + 64, cc * 128:(cc + 1) * 128],
                        Fb[fe][po:po + 64, 256 + cc:257 + cc],
                        PS[bc][po:po + 64, cc * 256 + hf * 128:cc * 256 + (hf + 1) * 128],
                        ALU.mult, ALU.add, ["S", ("F", fe), ("ps", bc)], ["S"])
                act(Sbf[:, :], Sst[:, :], AF.Copy, ["S"], ["Sbf"])
                act(Hb[OSQ][:, :], PS[bo][:, :], AF.Square, [("ps", bo)], [("H", OSQ)])
                tstate[t2] = bo

            def gla_tail(t2):
                t0 = g0 + t2 * 128
                tok = slice(t0, t0 + 128)
                Vb, KEb, STM, OSQ, TT_ = ((4, 5, 6, 7, 8), (11, 12, 13, 14, 15))[t2]
                FR = (2, 5)[t2]
                bo = tstate[t2]
                bs_ = nb()
                mm(bs_, PS[bs_][:, :], [(ones_bf[:, :], Hb[OSQ][:, :])], ["ones", ("H", OSQ)])
                act(Fb[FR][:, 0:512], PS[bs_][:, :], AF.Ln, [("ps", bs_), "eps"], [("F", FR)], bias=epsc[:, 0:1], scale=1.0 / 128)
                act(Fb[FR][:, 0:512], Fb[FR][:, 0:512], AF.Exp, [("F", FR)], [("F", FR)], scale=-0.5)
                tt(Hb[TT_][:, :], PS[bo][:, :], Fb[FR][:, 0:512], ALU.mult, [("ps", bo), ("F", FR)], [("H", TT_)])
                tt(BR[:, :, tok], v3(Hb[TT_][:, :], 4), BR[:, :, tok], ALU.mult, [("H", TT_), ("BR", st)], [("BR", st)])

            gla_front(0)
            gla_front(1)
            gla_mid(0)
            gla_mid(1)
            gla_tail(0)
            gla_tail(1)

        def out_stage(bi):
            s = next_slot()
            Wgt = SLOT[s][:, 0:NK * D].rearrange("p (k c) -> p k c", k=NK)
            c0 = 4112 + bi * 1024
            load_group(s, [(Wgt[:, k, :], w_in_d[l, k * 128:(k + 1) * 128, c0:c0 + 1024]) for k in range(NK)])
            dma("pool", [(SLW[:, kc, :], wA_d[bi][l, kc * 128:(kc + 1) * 128, :]) for kc in range(4)], ["SLW"], "slw")
            j = 0
            for st in range(NST):
                tsl = slice(st * 512, (st + 1) * 512)
                for dc in range(NK):
                    dsl = slice(dc * 128, (dc + 1) * 128)
                    by = nb()
                    mm(by, PS[by][:, :], [(SLW[:, kc, dsl], BR[:, kc, tsl]) for kc in range(4)], ["SLW", ("BR", st)])
                    bg = nb()
                    mm(bg, PS[bg][:, :], [(Wgt[:, k, dsl], hT[:, k, tsl]) for k in range(NK)], [("slot", s), ("hT", st)])
                    hb = 8 + (j % 2)
                    j += 1
                    act(Hb[hb][:, :], PS[bg][:, :], AF.Sigmoid, [("ps", bg)], [("H", hb)])
                    tt(Hb[dc][:, :], PS[by][:, :], Hb[hb][:, :], ALU.mult, [("ps", by), ("H", hb)], [("H", dc)])
                for dc in range(NK):
                    dsl = slice(dc * 128, (dc + 1) * 128)
                    b = nb()
                    mm(b, PS[b][:, :], [(WO[:, k, dsl], Hb[k][:, :]) for k in range(NK)], ["WO"] + [("H", k) for k in range(NK)])
                    tt(xT[:, dc, tsl], xT[:, dc, tsl], PS[b][:, :], ALU.add, [("xT", dc, st), ("ps", b)], [("xT", dc, st)])

        if STAGE < 4:
            continue
        out_stage(0)
        if STAGE < 5:
            continue

        s = next_slot()
        Ws = SLOT[s][:, 0:NK * 1024].rearrange("p (k c) -> p k c", k=NK)
        load_group(s, [(Ws[:, k, :], w_in_d[l, k * 128:(k + 1) * 128, 1552:2576]) for k in range(NK)])
        slr = ("slot", s)
        for st in range(NST):
            tsl = slice(st * 512, (st + 1) * 512)
            hres = ("hT", st)
            for c in range(4):
                b = nb()
                mm(b, PS[b][:, :], [(Ws[:, k, c * 128:(c + 1) * 128], hT[:, k, tsl]) for k in range(NK)], [slr, hres])
                act(Hb[c][:, :], PS[b][:, :], AF.Gelu_apprx_tanh, [("ps", b)], [("H", c)])
            def sgu_A(t4):
                t0 = st * 512 + t4 * 128
                tok = slice(t0, t0 + 128)
                p_ = t4 % 2
                f0_, f1_, h4_ = 3 * p_, 3 * p_ + 1, 4 + p_
                STAT, MV = STATS[p_], MVS[p_]
                b = nb()
                mm(b, PS[b][:, :], [(hT[:, k, tok], Ws[:, k, 512:1024]) for k in range(NK)], [slr, hres])
                act(Fb[f0_][:, 0:512], PS[b][:, :], AF.Gelu_apprx_tanh, [("ps", b)], [("F", f0_)])
                S.add("dve", lambda e, STAT=STAT, f0_=f0_: e.bn_stats(out=STAT[:, 0:6], in_=Fb[f0_][:, 0:512]),
                      reads=[("F", f0_)], writes=[("STAT", p_)])
                S.add("dve", lambda e, STAT=STAT, MV=MV: e.bn_aggr(out=MV[:, 0:2], in_=STAT[:, 0:6]),
                      reads=[("STAT", p_)], writes=[("MV", p_)])
                act(MV[:, 2:3], MV[:, 1:2], AF.Ln, [("MV", p_), "eps"], [("MV2", p_)], bias=epsc[:, 0:1], scale=1.0)
                act(MV[:, 3:4], MV[:, 2:3], AF.Exp, [("MV2", p_)], [("MV3", p_)], scale=-0.5)
                ts(Fb[f1_][:, 0:512], Fb[f0_][:, 0:512], MV[:, 0:1], MV[:, 3:4], ALU.subtract, ALU.mult,
                   [("F", f0_), ("MV", p_), ("MV3", p_)], [("F", f1_)])
                tt(Fb[f1_][:, 0:512], Fb[f1_][:, 0:512], LNG[:, :], ALU.mult, [("F", f1_), "LNG"], [("F", f1_)])
                tt(Hb[h4_][:, :], Fb[f1_][:, 0:512], LNB[:, :], ALU.add, [("F", f1_), "LNB"], [("H", h4_)])

            def sgu_B(t4):
                t0 = st * 512 + t4 * 128
                tok = slice(t0, t0 + 128)
                loc = slice(t4 * 128, (t4 + 1) * 128)
                p_ = t4 % 2
                f2_, h4_ = 3 * p_ + 2, 4 + p_
                bm = nb()
                for h in range(4):
                    hs = slice(h * 128, (h + 1) * 128)
                    mm(bm, PS[bm][:, hs], [(Hb[h4_][:, hs], WST[:, hs])], [("H", h4_), "WST"])
                tt(Fb[f2_][:, 0:512], PS[bm][:, :], BSB[:, :], ALU.add, [("ps", bm), "BSB"], [("F", f2_)])
                for h in range(4):
                    hs = slice(h * 128, (h + 1) * 128)
                    tt(BR[:, h, tok], Fb[f2_][:, hs], Hb[h][:, loc], ALU.mult, [("F", f2_), ("H", h)], [("BR", st)])

            sgu_A(0)
            sgu_A(1)
            sgu_B(0)
            sgu_A(2)
            sgu_B(1)
            sgu_A(3)
            sgu_B(2)
            sgu_B(3)
        if STAGE < 6:
            continue
        out_stage(1)
        if STAGE < 7:
            continue

        for cpair in range(2):
            s = next_slot()
            Wc = SLOT[s][:, 0:NK * 768].rearrange("p (k c) -> p k c", k=NK)
            prs = []
            for k in range(NK):
                for j3 in range(3):
                    c0 = 2576 + j3 * 512 + cpair * 256
                    prs.append((Wc[:, k, j3 * 256:(j3 + 1) * 256], w_in_d[l, k * 128:(k + 1) * 128, c0:c0 + 256]))
            load_group(s, prs)
            slr = ("slot", s)
            for ci in range(2):
                c = cpair * 2 + ci
                for st in range(NST):
                    tsl = slice(st * 512, (st + 1) * 512)
                    hres = ("hT", st)
                    pp = st % 2
                    bx, bc_, bb = nb(), nb(), nb()
                    mm(bx, PS[bx][:, :], [(Wc[:, k, 512 + ci * 128:512 + (ci + 1) * 128], hT[:, k, tsl]) for k in range(NK)], [slr, hres])
                    mm(bc_, PS[bc_][:, :], [(Wc[:, k, 256 + ci * 128:256 + (ci + 1) * 128], hT[:, k, tsl]) for k in range(NK)], [slr, hres])
                    mm(bb, PS[bb][:, :], [(Wc[:, k, ci * 128:(ci + 1) * 128], hT[:, k, tsl]) for k in range(NK)], [slr, hres])
                    act(Fb[2][:, 0:512], PS[bx][:, :], AF.Copy, [("ps", bx)], [("F", 2)])
                    if st == 0:
                        ms("dve", Fb[pp][:, 0:2], 0.0, [("F", pp)])
                    else:
                        cp("act", Fb[pp][:, 0:2], Fb[1 - pp][:, 512:514], [("F", 1 - pp)], [("F", pp)])
                    tt(Fb[pp][:, 2:514], PS[bc_][:, :], Fb[2][:, 0:512], ALU.mult, [("ps", bc_), ("F", 2)], [("F", pp)])
                    cw = lambda kk: cvl[:, 20 + kk * 4 + c:21 + kk * 4 + c]
                    ts(Fb[3][:, 0:512], Fb[pp][:, 2:514], cw(2), None, ALU.mult, None, [("F", pp), "cvl"], [("F", 3)])
                    stt(Fb[3][:, 0:512], Fb[pp][:, 1:513], cw(1), Fb[3][:, 0:512], ALU.mult, ALU.add, [("F", pp), ("F", 3), "cvl"], [("F", 3)])
                    stt(Fb[3][:, 0:512], Fb[pp][:, 0:512], cw(0), Fb[3][:, 0:512], ALU.mult, ALU.add, [("F", pp), ("F", 3), "cvl"], [("F", 3)])
                    tt(BR[:, c, tsl], Fb[3][:, 0:512], PS[bb][:, :], ALU.mult, [("F", 3), ("ps", bb)], [("BR", st)])
        if STAGE < 8:
            continue
        out_stage(2)
        if STAGE < 9:
            continue

        norm_to_h(8)
        groups = []
        f0 = 0
        while f0 < NF:
            G = min(4, NF - f0)
            groups.append((f0, G))
            f0 += G

        def load_up(gi):
            f0, G = groups[gi]
            s_ = next_slot()
            Wu_ = SLOT[s_][:, 0:NK * 1024].rearrange("p (k c) -> p k c", k=NK)
            prs_ = []
            for k in range(NK):
                prs_.append((Wu_[:, k, 0:G * 128], w_up_d[l, k * 128:(k + 1) * 128, f0 * 128:(f0 + G) * 128]))
                prs_.append((Wu_[:, k, 512:512 + G * 128], w_up_d[l, k * 128:(k + 1) * 128, DFF + f0 * 128:DFF + (f0 + G) * 128]))
            load_group(s_, prs_)
            return s_

        AG = [(Fb[4][:, 0:512], [("F", 4)]), (FX[0], [("H", 0), ("H", 1)])]
        AV = [(Fb[5][:, 0:512], [("F", 5)]), (FX[1], [("H", 2), ("H", 3)])]
        gslots = {0: load_up(0)}
        stepn = 0
        for gi, (f0, G) in enumerate(groups):
            if gi + 1 < len(groups):
                gslots[gi + 1] = load_up(gi + 1)
            s = gslots[gi]
            Wu = SLOT[s][:, 0:NK * 1024].rearrange("p (k c) -> p k c", k=NK)
            dma("pool", [(SLW[:, j, :], w_dn_d[l, (f0 + j) * 128:(f0 + j + 1) * 128, :]) for j in range(G)], ["SLW"], "slw")
            slr = ("slot", s)
            pending = None

            def flush(pd):
                par_, j_, st_ = pd
                tsl_ = slice(st_ * 512, (st_ + 1) * 512)
                hb_ = 8 + par_
                ag_, agr_ = AG[par_]
                av_, avr_ = AV[par_]
                act(Hb[hb_][:, :], ag_, AF.Silu, agr_, [("H", hb_)])
                tt(BR[:, j_, tsl_], Hb[hb_][:, :], av_, ALU.mult, [("H", hb_)] + avr_, [("BR", st_)], eng="pool")

            for j in range(G):
                f = f0 + j
                cg = lambda kk: cvl[:, 32 + kk * 44 + f:33 + kk * 44 + f]
                cvv = lambda kk: cvl[:, 32 + kk * 44 + 22 + f:33 + kk * 44 + 22 + f]
                bgc = cvl[:, 164 + f:165 + f]
                bvc = cvl[:, 164 + 22 + f:165 + 22 + f]
                for st in range(NST):
                    tsl = slice(st * 512, (st + 1) * 512)
                    hres = ("hT", st)
                    pp = st % 2
                    par = stepn % 2
                    stepn += 1
                    ag, agr = AG[par]
                    av, avr = AV[par]
                    ug, uv = pp, 2 + pp
                    bg, bv = nb(), nb()
                    mm(bg, PS[bg][:, :], [(Wu[:, k, j * 128:(j + 1) * 128], hT[:, k, tsl]) for k in range(NK)], [slr, hres])
                    mm(bv, PS[bv][:, :], [(Wu[:, k, 512 + j * 128:512 + (j + 1) * 128], hT[:, k, tsl]) for k in range(NK)], [slr, hres])
                    for (u, b_) in ((ug, bg), (uv, bv)):
                        if st == 0:
                            ms("dve", Fb[u][:, 0:2], 0.0, [("F", u)])
                        else:
                            uo = u - pp + (1 - pp)
                            cp("act", Fb[u][:, 0:2], Fb[uo][:, 512:514], [("F", uo)], [("F", u)])
                        act(Fb[u][:, 2:514], PS[b_][:, :], AF.Copy, [("ps", b_)], [("F", u)])
                    act(ag, PS[bg][:, :], AF.Identity, [("ps", bg), "cvl"], agr, bias=bgc, scale=cg(2))
                    ts(av, Fb[uv][:, 2:514], cvv(2), bvc, ALU.mult, ALU.add, [("F", uv), "cvl"], avr, eng="pool")
                    if pending is not None:
                        flush(pending)
                    for (u, a_, ar_, cw_) in ((ug, ag, agr, cg), (uv, av, avr, cvv)):
                        stt(a_, Fb[u][:, 1:513], cw_(1), a_, ALU.mult, ALU.add, [("F", u), "cvl"] + ar_, ar_)
                        stt(a_, Fb[u][:, 0:512], cw_(0), a_, ALU.mult, ALU.add, [("F", u), "cvl"] + ar_, ar_)
                    pending = (par, j, st)
            flush(pending)
            for st in range(NST):
                tsl = slice(st * 512, (st + 1) * 512)
                for dc in range(NK):
                    dsl = slice(dc * 128, (dc + 1) * 128)
                    b = nb()
                    mm(b, PS[b][:, :], [(SLW[:, j, dsl], BR[:, j, tsl]) for j in range(G)], ["SLW", ("BR", st)])
                    tt(xT[:, dc, tsl], xT[:, dc, tsl], PS[b][:, :], ALU.add, [("xT", dc, st), ("ps", b)], [("xT", dc, st)])

    finals = []
    for st in range(NST):
        tsl = slice(st * 512, (st + 1) * 512)
        rmsnorm_rstd(st, tsl)
        for k in range(NK):
            ob = 1 + (k % 4)
            stt(Fb[ob][:, 0:512], xT[:, k, tsl], cvf[:, k:k + 1], Fb[0][:, 0:512], ALU.mult, ALU.mult,
                [("xT", k, st), ("F", 0), "cvf"], [("F", ob)])
            finals.append(dma("sp", [(yT_d[k * 128:(k + 1) * 128, tsl], Fb[ob][:, 0:512])], [], "out%d" % ob,
                              reads=[("F", ob)]))
    S.emit(nc, es, final_waits=finals)
    es.close()
    return nc


def prep_inputs(inp, NL=4):
    f = lambda a: np.ascontiguousarray(np.asarray(a, dtype=np.float32))
    sh = {}
    sh["w_in"] = f(inp["w_in"][:NL])
    sh["gla_w_out"] = f(inp["gla_w_out"][:NL])
    sh["sgu_w_out"] = f(inp["sgu_w_out"][:NL])
    sh["conv_w_out"] = f(inp["conv_w_out"][:NL])
    sh["w_o"] = f(inp["w_o"][:NL])
    sh["ffn_w_up"] = f(inp["ffn_w_up"][:NL])
    sh["ffn_w_down"] = f(inp["ffn_w_down"][:NL])
    sh["walpha17"] = f(np.concatenate([np.asarray(inp["gla_w_alpha"])[:NL], np.asarray(inp["gla_b_alpha"])[:NL, None, :]], axis=1))
    sh["wsT"] = f(np.transpose(np.asarray(inp["sgu_ws"])[:NL], (0, 3, 1, 2)).reshape(NL, 128, 512))
    cv = np.zeros((128, NL * CV_L + 8), np.float32)
    pm = lambda v, n: np.asarray(v, dtype=np.float32).reshape(n, 128).T
    for l in range(NL):
        b = l * CV_L
        cv[:, b:b + 8] = pm(inp["norm_mix"][l], 8)
        cv[:, b + 8:b + 16] = pm(inp["norm_ffn"][l], 8)
        cv[:, b + 16:b + 20] = pm(inp["gla_norm"][l], 4)
        for kk in range(3):
            cv[:, b + 20 + kk * 4:b + 24 + kk * 4] = pm(inp["conv_w"][l][kk], 4)
            cv[:, b + 32 + kk * 44:b + 32 + (kk + 1) * 44] = pm(inp["ffn_conv_w"][l][kk], 44)
        cv[:, b + 164:b + 208] = pm(inp["ffn_conv_b"][l], 44)
    cv[:, NL * CV_L:NL * CV_L + 8] = pm(inp["norm_final"], 8)
    sh["cvec"] = cv
    rb = np.zeros((NL, 128, 1536), np.float32)
    for l in range(NL):
        rb[l, :, 0:512] = np.asarray(inp["sgu_ln_g"])[l][None, :]
        rb[l, :, 512:1024] = np.asarray(inp["sgu_ln_b"])[l][None, :]
        rb[l, :, 1024:1536] = np.asarray(inp["sgu_bs"])[l].reshape(1, 512)
    sh["rowb"] = rb
    j = np.arange(128)[:, None]
    i = np.arange(128)[None, :]
    cm = np.zeros((128, 768), np.float32)
    cm[:, 0:128] = np.where(j <= i, -1.0 / 16.0, 0.0)
    cm[:, 128:256] = np.where(j > i, -1.0 / 16.0, 0.0)
    cm[:, 256:768] = np.tile(np.where(j <= i, 1.0, 0.0), (1, 4))
    sh["cmat"] = cm
    return sh


_CACHE = {}


def run(inputs, T, NL, ncores):
    key = (T, NL)
    if key not in _CACHE:
        _CACHE[key] = build(T, NL)
    nc = _CACHE[key]
    shared = prep_inputs(inputs, NL)
    x = np.asarray(inputs["x"], dtype=np.float32)
    in_maps = []
    for c in range(ncores):
        m = dict(shared)
        m["xT"] = np.ascontiguousarray(x[c, :T].T)
        in_maps.append(m)
    res = run_bass_kernel_spmd(nc, in_maps, core_ids=list(range(ncores)))
    out = np.stack([np.ascontiguousarray(r["yT"].T) for r in res.results], axis=0)
    return out.astype(np.float32)


def kernel(**inputs):
    return run(inputs, 2048, 4, 8)
```

```python
from contextlib import ExitStack
import numpy as np
import concourse.bass as bass
import concourse.mybir as mybir
from concourse.bass_utils import run_bass_kernel_spmd

F32 = mybir.dt.float32
BF16 = mybir.dt.bfloat16
AF = mybir.ActivationFunctionType
ALU = mybir.AluOpType

D = 1024
NK = 8
INC = 7184
DFF = 2816
NF = 22
EPS = 1e-6
CV_L = 208


class _Op:
    __slots__ = ("eng", "fn", "deps", "dma", "ndma", "signal", "count", "sem")


class Sched:
    ENGS = ("pe", "act", "dve", "pool", "sp")

    def __init__(self):
        self.ops = []
        self.by_eng = {e: [] for e in self.ENGS}
        self.last_w = {}
        self.readers = {}

    def add(self, eng, fn, reads=(), writes=(), dma=None, ndma=1):
        op = _Op()
        op.eng, op.fn, op.dma, op.ndma = eng, fn, dma, ndma
        op.signal = dma is not None
        op.count = 0
        op.sem = None
        deps = {}

        def need(d, raw):
            if d.dma is not None or dma is not None:
                return True
            if d.eng != eng:
                return True
            if eng == "pe":
                return False
            return True

        for r in reads:
            w = self.last_w.get(r)
            if w is not None and need(w, True):
                deps[id(w)] = w
        for r in writes:
            w = self.last_w.get(r)
            if w is not None and need(w, False):
                deps[id(w)] = w
            for rd in self.readers.get(r, {}).values():
                if rd is not op and need(rd, False):
                    deps[id(rd)] = rd
        op.deps = list(deps.values())
        for d in op.deps:
            d.signal = True
        for r in reads:
            self.readers.setdefault(r, {})[eng if dma is None else ("dma", len(self.ops))] = op
        for r in writes:
            self.last_w[r] = op
            self.readers[r] = {}
        self.ops.append(op)
        self.by_eng[eng].append(op)
        return op

    def emit(self, nc, es, final_waits=()):
        sems = {}

        def get_sem(key):
            if key not in sems:
                sems[key] = es.enter_context(nc.semaphore("s_%s_%s" % key))
            return sems[key]

        cnt = {}
        for op in self.ops:
            if op.dma is not None:
                key = ("d", op.dma)
                cnt[key] = cnt.get(key, 0) + 16 * op.ndma
                op.sem, op.count = key, cnt[key]
            elif op.signal:
                key = ("e", op.eng)
                cnt[key] = cnt.get(key, 0) + 1
                op.sem, op.count = key, cnt[key]
        for k in cnt:
            get_sem(k)
        block = es.enter_context(nc.Block())
        sections = {"pe": block.tensor, "act": block.scalar, "dve": block.vector,
                    "pool": block.gpsimd, "sp": block.sync}

        def make(engname):
            oplist = self.by_eng[engname]

            def body(e):
                waited = {}
                for op in oplist:
                    need = {}
                    for d in op.deps:
                        if need.get(d.sem, 0) < d.count:
                            need[d.sem] = d.count
                    for k, v in need.items():
                        if waited.get(k, 0) < v:
                            e.wait_ge(sems[k], v)
                            waited[k] = v
                    if op.dma is not None:
                        op.fn(e, sems[op.sem])
                    else:
                        ins = op.fn(e)
                        if op.signal:
                            ins.then_inc(sems[op.sem], 1)
                if engname == "sp":
                    for op in final_waits:
                        e.wait_ge(sems[op.sem], op.count)
            return body

        for en in self.ENGS:
            sections[en](make(en))


STAGE = 99
SUB = 99


def build(T=2048, NL=4):
    NST = T // 512
    GST = 256
    nc = bass.Bass("TRN2", target_bir_lowering=False)
    es = ExitStack()

    def dram(name, shape, kind="ExternalInput"):
        return nc.dram_tensor(name, list(shape), F32, kind=kind).ap()

    xT_d = dram("xT", [D, T])
    w_in_d = dram("w_in", [NL, D, INC])
    wA_d = [dram(n, [NL, 512, D]) for n in ("gla_w_out", "sgu_w_out", "conv_w_out")]
    w_o_d = dram("w_o", [NL, D, D])
    w_up_d = dram("ffn_w_up", [NL, D, 2 * DFF])
    w_dn_d = dram("ffn_w_down", [NL, DFF, D])
    walpha_d = dram("walpha17", [NL, 17, 256])
    wsT_d = dram("wsT", [NL, 128, 512])
    cvec_d = dram("cvec", [128, NL * CV_L + 8])
    rowb_d = dram("rowb", [NL, 128, 1536])
    cmat_d = dram("cmat", [128, 768])
    yT_d = dram("yT", [D, T], kind="ExternalOutput")

    def sb(name, shape, dt):
        return es.enter_context(nc.sbuf_tensor(name, list(shape), dt))

    xT = sb("xT_s", [128, NK, T], F32)
    hT = sb("hT_s", [128, NK, T], BF16)
    BR = sb("BR_s", [128, 4, T], BF16)
    WO = sb("WO_s", [128, NK, D], BF16)
    SLOT_E = 8320
    SLOT = [sb("slot%d" % i, [128, SLOT_E], BF16) for i in range(2)]
    SLW = sb("slotW", [128, 4, D], BF16)
    NFB, NHB = 6, 16
    Fb = [sb("F%d" % i, [128, 516], F32) for i in range(NFB)]
    HP = [sb("HP%d" % i, [128, 1024], BF16) for i in range(NHB // 2)]
    Hb = [HP[i // 2][:, (i % 2) * 512:(i % 2 + 1) * 512] for i in range(NHB)]
    FX = [HP[i][:, :].bitcast(F32) for i in range(NHB // 2)]
    cvf = sb("cvf", [128, 8], F32)
    cvl = sb("cvl", [128, CV_L], F32)
    triU = sb("triU", [128, 128], BF16)
    triSU = sb("triSU", [128, 128], BF16)
    MASK = sb("MASK", [128, 512], BF16)
    ones_bf = sb("ones_bf", [128, 128], BF16)
    epsc = sb("epsc", [128, 1], F32)
    A17 = sb("A17", [32, GST], BF16)
    walpha = sb("walpha", [32, 256], BF16)
    WST = sb("WST", [128, 512], BF16)
    LNG = sb("LNG", [128, 512], BF16)
    LNB = sb("LNB", [128, 512], BF16)
    BSB = sb("BSB", [128, 512], F32)
    Sst = sb("Sst", [128, 256], F32)
    Sbf = sb("Sbf", [128, 256], BF16)
    DEC = sb("DEC", [128, 2], F32)
    STATS = [sb("STAT%d" % i, [128, 8], F32) for i in range(2)]
    MVS = [sb("MV%d" % i, [128, 4], F32) for i in range(2)]
    PS = [es.enter_context(nc.psum_tensor("ps%d" % i, [128, 512], F32)) for i in range(8)]

    S = Sched()
    bank_ctr = [0]

    def nb():
        b = bank_ctr[0] % 8
        bank_ctr[0] += 1
        return b

    def mm(bank, out_ap, pairs, reads):
        def fn(pe):
            n = len(pairs)
            ins = None
            for i, (l, r) in enumerate(pairs):
                ins = pe.matmul(out_ap, l, r, start=(i == 0), stop=(i == n - 1))
            return ins
        return S.add("pe", fn, reads=reads, writes=[("ps", bank)])

    def act(out, in_, func, reads, writes, bias=None, scale=None):
        kw = {}
        if bias is not None:
            kw["bias"] = bias
        if scale is not None:
            kw["scale"] = scale
        return S.add("act", lambda e: e.activation(out=out, in_=in_, func=func, **kw), reads=reads, writes=writes)

    def tt(out, in0, in1, op, reads, writes, eng="dve"):
        return S.add(eng, lambda e: e.tensor_tensor(out=out, in0=in0, in1=in1, op=op), reads=reads, writes=writes)

    def ts(out, in0, s1, s2, op0, op1, reads, writes, eng="dve"):
        if s2 is None:
            return S.add(eng, lambda e: e.tensor_scalar(out=out, in0=in0, scalar1=s1, scalar2=None, op0=op0),
                         reads=reads, writes=writes)
        return S.add(eng, lambda e: e.tensor_scalar(out=out, in0=in0, scalar1=s1, scalar2=s2, op0=op0, op1=op1),
                     reads=reads, writes=writes)

    def stt(out, in0, scalar, in1, op0, op1, reads, writes, eng="dve"):
        return S.add(eng, lambda e: e.scalar_tensor_tensor(out=out, in0=in0, scalar=scalar, in1=in1, op0=op0, op1=op1),
                     reads=reads, writes=writes)

    def cp(eng, out, in_, reads, writes):
        if eng == "act":
            return S.add(eng, lambda e: e.activation(out=out, in_=in_, func=AF.Copy), reads=reads, writes=writes)
        return S.add(eng, lambda e: e.tensor_copy(out=out, in_=in_), reads=reads, writes=writes)

    def ms(eng, ap, val, writes):
        return S.add(eng, lambda e: e.memset(ap, val), reads=[], writes=writes)

    def dma(eng, pairs, writes, semkey, reads=()):
        def fn(e, sem):
            for o, i in pairs:
                e.dma_start(out=o, in_=i).then_inc(sem, 16)
        return S.add(eng, fn, reads=reads, writes=writes, dma=semkey, ndma=len(pairs))

    def v3(ap, a):
        return ap.rearrange("p (a b) -> p a b", a=a)

    dma("sp", [(xT[:, k, :], xT_d[k * 128:(k + 1) * 128, :]) for k in range(NK)],
        [("xT", k, st) for k in range(NK) for st in range(NST)], "init")
    dma("sp", [(cvf[:, :], cvec_d[:, NL * CV_L:NL * CV_L + 8])], ["cvf"], "init2")
    dma("pool", [(MASK[:, :], cmat_d[:, 256:768]), (triU[:, :], cmat_d[:, 0:128]), (triSU[:, :], cmat_d[:, 128:256])],
        ["MASK", "tri"], "initp")
    ms("dve", ones_bf[:, :], 1.0, ["ones"])
    ms("dve", epsc[:, :], EPS, ["eps"])
    ms("dve", A17[:, :], 1.0, ["A17"])

    def rmsnorm_rstd(st, tsl, sqb=(8, 9)):
        b = nb()
        sqs = []
        for k in range(NK):
            hb = Hb[sqb[k % 2]]
            hres = ("H", sqb[k % 2])
            act(hb[:, :], xT[:, k, tsl], AF.Square, [("xT", k, st)], [hres])
            sqs.append((hb, hres))
            def fn(pe, k=k, hb=hb, b=b):
                return pe.matmul(PS[b][:, :], ones_bf[:, :], hb[:, :], start=(k == 0), stop=(k == NK - 1))
            S.add("pe", fn, reads=[hres, "ones"], writes=[("ps", b)])
        act(Fb[0][:, 0:512], PS[b][:, :], AF.Ln, [("ps", b), "eps"], [("F", 0)], bias=epsc[:, 0:1], scale=1.0 / D)
        act(Fb[0][:, 0:512], Fb[0][:, 0:512], AF.Exp, [("F", 0)], [("F", 0)], scale=-0.5)
        return 0

    def norm_st(l_col, st, sqb=(8, 9)):
        tsl = slice(st * 512, (st + 1) * 512)
        rmsnorm_rstd(st, tsl, sqb)
        for k in range(NK):
            stt(hT[:, k, tsl], xT[:, k, tsl], cvl[:, l_col + k:l_col + k + 1], Fb[0][:, 0:512],
                ALU.mult, ALU.mult, [("xT", k, st), ("F", 0), "cvl"], [("hT", st)])

    def norm_to_h(l_col):
        for st in range(NST):
            norm_st(l_col, st)

    slot_ctr = [0]

    def next_slot():
        s = slot_ctr[0] % 2
        slot_ctr[0] += 1
        return s

    def load_group(s, pairs):
        dma("pool", pairs, [("slot", s)], "slot%d" % s)

    for l in range(NL):
        dma("sp", [(cvl[:, :], cvec_d[:, l * CV_L:(l + 1) * CV_L]),
                   (BSB[:, :], rowb_d[l, :, 1024:1536])], ["cvl", "BSB"], "lsmall")
        dma("pool", [(WST[:, :], wsT_d[l]), (LNG[:, :], rowb_d[l, :, 0:512]), (LNB[:, :], rowb_d[l, :, 512:1024]),
                     (walpha[0:17, :], walpha_d[l])],
            ["WST", "LNG", "LNB", "walpha"], "lsmallp")
        tt(WST[:, :], WST[:, :], MASK[:, :], ALU.mult, ["WST", "MASK"], ["WST"])
        dma("pool", [(WO[:, k, :], w_o_d[l, k * 128:(k + 1) * 128, :]) for k in range(NK)], ["WO"], "wo")

        if STAGE < 1:
            continue
        norm_to_h(0)
        if STAGE < 2:
            continue

        s = next_slot()
        Wg = SLOT[s][:, 0:NK * 512].rearrange("p (k c) -> p k c", k=NK)
        load_group(s, [(Wg[:, k, :], w_in_d[l, k * 128:(k + 1) * 128, 1024:1536]) for k in range(NK)])
        i = 0
        for st in range(NST):
            tsl = slice(st * 512, (st + 1) * 512)
            for h in range(4):
                b = nb()
                mm(b, PS[b][:, :], [(Wg[:, k, h * 128:(h + 1) * 128], hT[:, k, tsl]) for k in range(NK)],
                   [("slot", s), ("hT", st)])
                hb = 8 + (i % 2)
                i += 1
                act(Hb[hb][:, :], PS[b][:, :], AF.Sigmoid, [("ps", b)], [("H", hb)])
                stt(BR[:, h, tsl], PS[b][:, :], cvl[:, 16 + h:17 + h], Hb[hb][:, :], ALU.mult, ALU.mult,
                    [("ps", b), ("H", hb), "cvl"], [("BR", st)])

        if STAGE < 3:
            continue
        s = next_slot()
        Wm = SLOT[s][:, 0:NK * 1040].rearrange("p (k c) -> p k c", k=NK)
        prs = []
        for k in range(NK):
            prs.append((Wm[:, k, 0:1024], w_in_d[l, k * 128:(k + 1) * 128, 0:1024]))
            prs.append((Wm[:, k, 1024:1040], w_in_d[l, k * 128:(k + 1) * 128, 1536:1552]))
        load_group(s, prs)
        slr = ("slot", s)
        ms("dve", Sst[:, :], 0.0, ["S"])
        ms("dve", Sbf[:, :], 0.0, ["Sbf"])
        EB, ENB, QT, KT, Vb, KEb, STM, OSQ, TT_ = 0, 1, 2, 3, 4, 5, 6, 7, 8
        QTO = 10
        ms("dve", Hb[QT][:, :], 0.0, [("H", QT)])
        ms("dve", Hb[QTO][:, :], 0.0, [("H", QTO)])
        FELA, FR = 1, 2
        for gst in range(T // GST):
            g0 = gst * GST
            gsl = slice(g0, g0 + GST)
            st = g0 // 512
            hres = ("hT", st)
            b = nb()
            mm(b, PS[b][0:16, 0:GST], [(Wm[:, k, 1024:1040], hT[:, k, gsl]) for k in range(NK)], [slr, hres])
            if SUB < 1:
                continue
            act(A17[0:16, :], PS[b][0:16, 0:GST], AF.Copy, [("ps", b)], ["A17"])
            if SUB < 2:
                continue
            for t2 in range(GST // 128):
                loc = slice(t2 * 128, (t2 + 1) * 128)
                b = nb()
                mm(b, PS[b][:, 0:256], [(A17[0:17, loc], walpha[0:17, :])], ["A17", "walpha"])
                act(Fb[FELA][:, 0:256], PS[b][:, 0:256], AF.Exp, [("ps", b)], [("F", FELA)], scale=-1.0)
                act(Fb[FELA][:, 256:512], Fb[FELA][:, 0:256], AF.Ln, [("F", FELA)], [("F", FELA)], bias=1.0, scale=1.0)
                la = Fb[FELA]
                LAH, LAL = Hb[9][:, 0:256], Hb[9][:, 256:512]
                cp("dve", LAH, la[:, 256:512], [("F", FELA)], [("H", 9)])
                tt(LAL, la[:, 256:512], LAH, ALU.subtract, [("F", FELA), ("H", 9)], [("H", 9)])
                if SUB < 3:
                    continue
                b = nb()
                for cc in range(2):
                    mm(b, PS[b][:, cc * 128:(cc + 1) * 128],
                       [(LAH[:, cc * 128:(cc + 1) * 128], triU[:, :]), (LAL[:, cc * 128:(cc + 1) * 128], triU[:, :])],
                       [("H", 9), "tri"])
                mm(b, PS[b][:, 256:512], [(triSU[:, :], LAH), (triSU[:, :], LAL)], [("H", 9), "tri"])
                for cc in range(2):
                    act(Hb[EB][:, cc * GST + t2 * 128:cc * GST + (t2 + 1) * 128], PS[b][:, cc * 128:(cc + 1) * 128],
                        AF.Exp, [("ps", b)], [("H", EB)])
                    act(Hb[ENB][:, cc * GST + t2 * 128:cc * GST + (t2 + 1) * 128], PS[b][:, cc * 128:(cc + 1) * 128],
                        AF.Exp, [("ps", b)], [("H", ENB)], scale=-1.0)
                if SUB < 4:
                    continue
                act(Fb[3 + t2][:, 0:256], PS[b][:, 256:512], AF.Exp, [("ps", b)], [("F", 3 + t2)])
                act(Fb[3 + t2][:, 256:258], PS[b][:, 127:256:128], AF.Exp, [("ps", b)], [("F", 3 + t2)])
            if SUB < 5:
                continue
            for cc in range(2):
                b = nb()
                mm(b, PS[b][:, 0:GST], [(Wm[:, k, cc * 128:(cc + 1) * 128], hT[:, k, gsl]) for k in range(NK)], [slr, hres])
                stt(Hb[QT][0:64, cc * GST:(cc + 1) * GST], PS[b][0:64, 0:GST], 0.125, Hb[EB][0:64, cc * GST:(cc + 1) * GST],
                    ALU.mult, ALU.mult, [("ps", b), ("H", EB)], [("H", QT)])
                stt(Hb[QTO][64:128, cc * GST:(cc + 1) * GST], PS[b][64:128, 0:GST], 0.125, Hb[EB][64:128, cc * GST:(cc + 1) * GST],
                    ALU.mult, ALU.mult, [("ps", b), ("H", EB)], [("H", QTO)])
                b = nb()
                mm(b, PS[b][:, 0:GST], [(Wm[:, k, 256 + cc * 128:256 + (cc + 1) * 128], hT[:, k, gsl]) for k in range(NK)],
                   [slr, hres])
                tt(Hb[KT][:, cc * GST:(cc + 1) * GST], PS[b][:, 0:GST], Hb[ENB][:, cc * GST:(cc + 1) * GST], ALU.mult,
                   [("ps", b), ("H", ENB)], [("H", KT)])
            tstate = {}

            def gla_front(t2):
                t0 = g0 + t2 * 128
                tok = slice(t0, t0 + 128)
                fe = 3 + t2
                Vb, KEb, STM, OSQ, TT_ = ((4, 5, 6, 7, 8), (11, 12, 13, 14, 15))[t2]
                b = nb()
                mm(b, PS[b][:, :], [(hT[:, k, tok], Wm[:, k, 512:1024]) for k in range(NK)], [slr, hres])
                act(Hb[Vb][:, :], PS[b][:, :], AF.Copy, [("ps", b)], [("H", Vb)])
                b = nb()
                mm(b, PS[b][:, 0:256], [(hT[:, k, tok], Wm[:, k, 256:512]) for k in range(NK)], [slr, hres])
                tt(Hb[KEb][:, 0:256], PS[b][:, 0:256], Fb[fe][:, 0:256], ALU.mult, [("ps", b), ("F", fe)], [("H", KEb)])
                b = nb()
                for h in range(4):
                    cc = h // 2
                    c0 = cc * GST + t2 * 128
                    qz = QT if h % 2 == 0 else QTO
                    mm(b, PS[b][:, h * 128:(h + 1) * 128],
                       [(Hb[KT][:, c0:c0 + 128], Hb[qz][:, c0:c0 + 128])], [("H", KT), ("H", qz)])
                tt(Hb[STM][:, :], PS[b][:, :], MASK[:, :], ALU.mult, [("ps", b), "MASK"], [("H", STM)])

            def gla_mid(t2):
                fe = 3 + t2
                Vb, KEb, STM, OSQ, TT_ = ((4, 5, 6, 7, 8), (11, 12, 13, 14, 15))[t2]
                bo = nb()
                for h in range(4):
                    cc = h // 2
                    c0 = cc * GST + t2 * 128
                    qz = QT if h % 2 == 0 else QTO
                    mm(bo, PS[bo][:, h * 128:(h + 1) * 128],
                       [(Sbf[:, cc * 128:(cc + 1) * 128], Hb[qz][:, c0:c0 + 128]),
                        (Hb[Vb][:, h * 128:(h + 1) * 128], Hb[STM][:, h * 128:(h + 1) * 128])],
                       ["Sbf", ("H", qz), ("H", Vb), ("H", STM)])
                bc = nb()
                for cc in range(2):
                    mm(bc, PS[bc][:, cc * 256:(cc + 1) * 256],
                       [(Hb[KEb][:, cc * 128:(cc + 1) * 128], Hb[Vb][:, cc * 256:(cc + 1) * 256])], [("H", KEb), ("H", Vb)])
                for h in range(4):
                    cc, po = h // 2, (h % 2) * 64
                    hf = h % 2
                    stt(Sst[po:po + 64, cc * 128:(cc + 1) * 128], Sst[po:po + 64, cc * 128:(cc + 1) * 128],
                        Fb[fe][po:po + 64, 256 + cc:257 + cc],
                        PS[bc][po:po + 64, cc * 256 + hf * 128:cc * 256 + (hf + 1) * 128],
                        ALU.mult, ALU.add, ["S", ("F", fe), ("ps", bc)], ["S"])
                act(Sbf[:, :], Sst[:, :], AF.Copy, ["S"], ["Sbf"])
                act(Hb[OSQ][:, :], PS[bo][:, :], AF.Square, [("ps", bo)], [("H", OSQ)])
                tstate[t2] = bo

            def gla_tail(t2):
                t0 = g0 + t2 * 128
                tok = slice(t0, t0 + 128)
                Vb, KEb, STM, OSQ, TT_ = ((4, 5, 6, 7, 8), (11, 12, 13, 14, 15))[t2]
                FR = (2, 5)[t2]
                bo = tstate[t2]
                bs_ = nb()
                mm(bs_, PS[bs_][:, :], [(ones_bf[:, :], Hb[OSQ][:, :])], ["ones", ("H", OSQ)])
                act(Fb[FR][:, 0:512], PS[bs_][:, :], AF.Ln, [("ps", bs_), "eps"], [("F", FR)], bias=epsc[:, 0:1], scale=1.0 / 128)
                act(Fb[FR][:, 0:512], Fb[FR][:, 0:512], AF.Exp, [("F", FR)], [("F", FR)], scale=-0.5)
                tt(Hb[TT_][:, :], PS[bo][:, :], Fb[FR][:, 0:512], ALU.mult, [("ps", bo), ("F", FR)], [("H", TT_)])
                tt(BR[:, :, tok], v3(Hb[TT_][:, :], 4), BR[:, :, tok], ALU.mult, [("H", TT_), ("BR", st)], [("BR", st)])

            gla_front(0)
            gla_front(1)
            gla_mid(0)
            gla_mid(1)
            gla_tail(0)
            gla_tail(1)

        def out_stage(bi, post=None):
            s = next_slot()
            Wgt = SLOT[s][:, 0:NK * D].rearrange("p (k c) -> p k c", k=NK)
            c0 = 4112 + bi * 1024
            load_group(s, [(Wgt[:, k, :], w_in_d[l, k * 128:(k + 1) * 128, c0:c0 + 1024]) for k in range(NK)])
            dma("pool", [(SLW[:, kc, :], wA_d[bi][l, kc * 128:(kc + 1) * 128, :]) for kc in range(4)], ["SLW"], "slw")
            j = 0
            for st in range(NST):
                tsl = slice(st * 512, (st + 1) * 512)
                for dc in range(NK):
                    dsl = slice(dc * 128, (dc + 1) * 128)
                    by = nb()
                    mm(by, PS[by][:, :], [(SLW[:, kc, dsl], BR[:, kc, tsl]) for kc in range(4)], ["SLW", ("BR", st)])
                    bg = nb()
                    mm(bg, PS[bg][:, :], [(Wgt[:, k, dsl], hT[:, k, tsl]) for k in range(NK)], [("slot", s), ("hT", st)])
                    hb = 8 + (j % 2)
                    j += 1
                    act(Hb[hb][:, :], PS[bg][:, :], AF.Sigmoid, [("ps", bg)], [("H", hb)])
                    tt(Hb[dc][:, :], PS[by][:, :], Hb[hb][:, :], ALU.mult, [("ps", by), ("H", hb)], [("H", dc)])
                for dc in range(NK):
                    dsl = slice(dc * 128, (dc + 1) * 128)
                    b = nb()
                    mm(b, PS[b][:, :], [(WO[:, k, dsl], Hb[k][:, :]) for k in range(NK)], ["WO"] + [("H", k) for k in range(NK)])
                    tt(xT[:, dc, tsl], xT[:, dc, tsl], PS[b][:, :], ALU.add, [("xT", dc, st), ("ps", b)], [("xT", dc, st)])
                if post is not None:
                    post(st)

        if STAGE < 4:
            continue
        out_stage(0)
        if STAGE < 5:
            continue

        s = next_slot()
        Ws = SLOT[s][:, 0:NK * 1024].rearrange("p (k c) -> p k c", k=NK)
        load_group(s, [(Ws[:, k, :], w_in_d[l, k * 128:(k + 1) * 128, 1552:2576]) for k in range(NK)])
        slr = ("slot", s)
        for st in range(NST):
            tsl = slice(st * 512, (st + 1) * 512)
            hres = ("hT", st)
            for c in range(4):
                b = nb()
                mm(b, PS[b][:, :], [(Ws[:, k, c * 128:(c + 1) * 128], hT[:, k, tsl]) for k in range(NK)], [slr, hres])
                act(Hb[c][:, :], PS[b][:, :], AF.Gelu_apprx_tanh, [("ps", b)], [("H", c)])
            def sgu_A(t4):
                t0 = st * 512 + t4 * 128
                tok = slice(t0, t0 + 128)
                p_ = t4 % 2
                f0_, f1_, h4_ = 3 * p_, 3 * p_ + 1, 4 + p_
                STAT, MV = STATS[p_], MVS[p_]
                b = nb()
                mm(b, PS[b][:, :], [(hT[:, k, tok], Ws[:, k, 512:1024]) for k in range(NK)], [slr, hres])
                act(Fb[f0_][:, 0:512], PS[b][:, :], AF.Gelu_apprx_tanh, [("ps", b)], [("F", f0_)])
                S.add("dve", lambda e, STAT=STAT, f0_=f0_: e.bn_stats(out=STAT[:, 0:6], in_=Fb[f0_][:, 0:512]),
                      reads=[("F", f0_)], writes=[("STAT", p_)])
                S.add("dve", lambda e, STAT=STAT, MV=MV: e.bn_aggr(out=MV[:, 0:2], in_=STAT[:, 0:6]),
                      reads=[("STAT", p_)], writes=[("MV", p_)])
                act(MV[:, 2:3], MV[:, 1:2], AF.Ln, [("MV", p_), "eps"], [("MV2", p_)], bias=epsc[:, 0:1], scale=1.0)
                act(MV[:, 3:4], MV[:, 2:3], AF.Exp, [("MV2", p_)], [("MV3", p_)], scale=-0.5)
                ts(Fb[f1_][:, 0:512], Fb[f0_][:, 0:512], MV[:, 0:1], MV[:, 3:4], ALU.subtract, ALU.mult,
                   [("F", f0_), ("MV", p_), ("MV3", p_)], [("F", f1_)])
                tt(Fb[f1_][:, 0:512], Fb[f1_][:, 0:512], LNG[:, :], ALU.mult, [("F", f1_), "LNG"], [("F", f1_)])
                tt(Hb[h4_][:, :], Fb[f1_][:, 0:512], LNB[:, :], ALU.add, [("F", f1_), "LNB"], [("H", h4_)])

            def sgu_B(t4):
                t0 = st * 512 + t4 * 128
                tok = slice(t0, t0 + 128)
                loc = slice(t4 * 128, (t4 + 1) * 128)
                p_ = t4 % 2
                f2_, h4_ = 3 * p_ + 2, 4 + p_
                bm = nb()
                for h in range(4):
                    hs = slice(h * 128, (h + 1) * 128)
                    mm(bm, PS[bm][:, hs], [(Hb[h4_][:, hs], WST[:, hs])], [("H", h4_), "WST"])
                tt(Fb[f2_][:, 0:512], PS[bm][:, :], BSB[:, :], ALU.add, [("ps", bm), "BSB"], [("F", f2_)])
                for h in range(4):
                    hs = slice(h * 128, (h + 1) * 128)
                    tt(BR[:, h, tok], Fb[f2_][:, hs], Hb[h][:, loc], ALU.mult, [("F", f2_), ("H", h)], [("BR", st)])

            sgu_A(0)
            sgu_A(1)
            sgu_B(0)
            sgu_A(2)
            sgu_B(1)
            sgu_A(3)
            sgu_B(2)
            sgu_B(3)
        if STAGE < 6:
            continue
        out_stage(1)
        if STAGE < 7:
            continue

        for cpair in range(2):
            s = next_slot()
            Wc = SLOT[s][:, 0:NK * 768].rearrange("p (k c) -> p k c", k=NK)
            prs = []
            for k in range(NK):
                for j3 in range(3):
                    c0 = 2576 + j3 * 512 + cpair * 256
                    prs.append((Wc[:, k, j3 * 256:(j3 + 1) * 256], w_in_d[l, k * 128:(k + 1) * 128, c0:c0 + 256]))
            load_group(s, prs)
            slr = ("slot", s)
            for ci in range(2):
                c = cpair * 2 + ci
                for st in range(NST):
                    tsl = slice(st * 512, (st + 1) * 512)
                    hres = ("hT", st)
                    pp = st % 2
                    bx, bc_, bb = nb(), nb(), nb()
                    mm(bx, PS[bx][:, :], [(Wc[:, k, 512 + ci * 128:512 + (ci + 1) * 128], hT[:, k, tsl]) for k in range(NK)], [slr, hres])
                    mm(bc_, PS[bc_][:, :], [(Wc[:, k, 256 + ci * 128:256 + (ci + 1) * 128], hT[:, k, tsl]) for k in range(NK)], [slr, hres])
                    mm(bb, PS[bb][:, :], [(Wc[:, k, ci * 128:(ci + 1) * 128], hT[:, k, tsl]) for k in range(NK)], [slr, hres])
                    act(Fb[2][:, 0:512], PS[bx][:, :], AF.Copy, [("ps", bx)], [("F", 2)])
                    if st == 0:
                        ms("dve", Fb[pp][:, 0:2], 0.0, [("F", pp)])
                    else:
                        cp("act", Fb[pp][:, 0:2], Fb[1 - pp][:, 512:514], [("F", 1 - pp)], [("F", pp)])
                    tt(Fb[pp][:, 2:514], PS[bc_][:, :], Fb[2][:, 0:512], ALU.mult, [("ps", bc_), ("F", 2)], [("F", pp)])
                    cw = lambda kk: cvl[:, 20 + kk * 4 + c:21 + kk * 4 + c]
                    ts(Fb[3][:, 0:512], Fb[pp][:, 2:514], cw(2), None, ALU.mult, None, [("F", pp), "cvl"], [("F", 3)])
                    stt(Fb[3][:, 0:512], Fb[pp][:, 1:513], cw(1), Fb[3][:, 0:512], ALU.mult, ALU.add, [("F", pp), ("F", 3), "cvl"], [("F", 3)])
                    stt(Fb[3][:, 0:512], Fb[pp][:, 0:512], cw(0), Fb[3][:, 0:512], ALU.mult, ALU.add, [("F", pp), ("F", 3), "cvl"], [("F", 3)])
                    tt(BR[:, c, tsl], Fb[3][:, 0:512], PS[bb][:, :], ALU.mult, [("F", 3), ("ps", bb)], [("BR", st)])
        if STAGE < 8:
            continue
        out_stage(2, post=lambda st_: norm_st(8, st_, (10, 11)))
        if STAGE < 9:
            continue

        groups = []
        f0 = 0
        while f0 < NF:
            G = min(4, NF - f0)
            groups.append((f0, G))
            f0 += G

        def load_up(gi):
            f0, G = groups[gi]
            s_ = next_slot()
            Wu_ = SLOT[s_][:, 0:NK * 1024].rearrange("p (k c) -> p k c", k=NK)
            prs_ = []
            for k in range(NK):
                prs_.append((Wu_[:, k, 0:G * 128], w_up_d[l, k * 128:(k + 1) * 128, f0 * 128:(f0 + G) * 128]))
                prs_.append((Wu_[:, k, 512:512 + G * 128], w_up_d[l, k * 128:(k + 1) * 128, DFF + f0 * 128:DFF + (f0 + G) * 128]))
            load_group(s_, prs_)
            return s_

        AG = [(Fb[4][:, 0:512], [("F", 4)]), (FX[0], [("H", 0), ("H", 1)])]
        AV = [(Fb[5][:, 0:512], [("F", 5)]), (FX[1], [("H", 2), ("H", 3)])]
        gslots = {0: load_up(0)}
        stepn = 0
        for gi, (f0, G) in enumerate(groups):
            if gi + 1 < len(groups):
                gslots[gi + 1] = load_up(gi + 1)
            s = gslots[gi]
            Wu = SLOT[s][:, 0:NK * 1024].rearrange("p (k c) -> p k c", k=NK)
            dma("pool", [(SLW[:, j, :], w_dn_d[l, (f0 + j) * 128:(f0 + j + 1) * 128, :]) for j in range(G)], ["SLW"], "slw")
            slr = ("slot", s)
            pending = None

            def flush(pd):
                par_, j_, st_ = pd
                tsl_ = slice(st_ * 512, (st_ + 1) * 512)
                hb_ = 8 + par_
                ag_, agr_ = AG[par_]
                av_, avr_ = AV[par_]
                act(Hb[hb_][:, :], ag_, AF.Silu, agr_, [("H", hb_)])
                tt(BR[:, j_, tsl_], Hb[hb_][:, :], av_, ALU.mult, [("H", hb_)] + avr_, [("BR", st_)], eng="pool")

            for j in range(G):
                f = f0 + j
                cg = lambda kk: cvl[:, 32 + kk * 44 + f:33 + kk * 44 + f]
                cvv = lambda kk: cvl[:, 32 + kk * 44 + 22 + f:33 + kk * 44 + 22 + f]
                bgc = cvl[:, 164 + f:165 + f]
                bvc = cvl[:, 164 + 22 + f:165 + 22 + f]
                for st in range(NST):
                    tsl = slice(st * 512, (st + 1) * 512)
                    hres = ("hT", st)
                    pp = st % 2
                    par = stepn % 2
                    stepn += 1
                    ag, agr = AG[par]
                    av, avr = AV[par]
                    ug, uv = pp, 2 + pp
                    bg, bv = nb(), nb()
                    mm(bg, PS[bg][:, :], [(Wu[:, k, j * 128:(j + 1) * 128], hT[:, k, tsl]) for k in range(NK)], [slr, hres])
                    mm(bv, PS[bv][:, :], [(Wu[:, k, 512 + j * 128:512 + (j + 1) * 128], hT[:, k, tsl]) for k in range(NK)], [slr, hres])
                    for (u, b_) in ((ug, bg), (uv, bv)):
                        if st == 0:
                            ms("dve", Fb[u][:, 0:2], 0.0, [("F", u)])
                        else:
                            uo = u - pp + (1 - pp)
                            cp("act", Fb[u][:, 0:2], Fb[uo][:, 512:514], [("F", uo)], [("F", u)])
                        act(Fb[u][:, 2:514], PS[b_][:, :], AF.Copy, [("ps", b_)], [("F", u)])
                    act(ag, PS[bg][:, :], AF.Identity, [("ps", bg), "cvl"], agr, bias=bgc, scale=cg(2))
                    ts(av, Fb[uv][:, 2:514], cvv(2), bvc, ALU.mult, ALU.add, [("F", uv), "cvl"], avr, eng="pool")
                    if pending is not None:
                        flush(pending)
                    for (u, a_, ar_, cw_) in ((ug, ag, agr, cg), (uv, av, avr, cvv)):
                        stt(a_, Fb[u][:, 1:513], cw_(1), a_, ALU.mult, ALU.add, [("F", u), "cvl"] + ar_, ar_)
                        stt(a_, Fb[u][:, 0:512], cw_(0), a_, ALU.mult, ALU.add, [("F", u), "cvl"] + ar_, ar_)
                    pending = (par, j, st)
            flush(pending)
            for st in range(NST):
                tsl = slice(st * 512, (st + 1) * 512)
                for dc in range(NK):
                    dsl = slice(dc * 128, (dc + 1) * 128)
                    b = nb()
                    mm(b, PS[b][:, :], [(SLW[:, j, dsl], BR[:, j, tsl]) for j in range(G)], ["SLW", ("BR", st)])
                    tt(xT[:, dc, tsl], xT[:, dc, tsl], PS[b][:, :], ALU.add, [("xT", dc, st), ("ps", b)], [("xT", dc, st)])

    finals = []
    for st in range(NST):
        tsl = slice(st * 512, (st + 1) * 512)
        rmsnorm_rstd(st, tsl)
        for k in range(NK):
            ob = 1 + (k % 4)
            stt(Fb[ob][:, 0:512], xT[:, k, tsl], cvf[:, k:k + 1], Fb[0][:, 0:512], ALU.mult, ALU.mult,
                [("xT", k, st), ("F", 0), "cvf"], [("F", ob)])
            finals.append(dma("sp", [(yT_d[k * 128:(k + 1) * 128, tsl], Fb[ob][:, 0:512])], [], "out%d" % ob,
                              reads=[("F", ob)]))
    S.emit(nc, es, final_waits=finals)
    es.close()
    return nc


def prep_inputs(inp, NL=4):
    f = lambda a: np.ascontiguousarray(np.asarray(a, dtype=np.float32))
    sh = {}
    sh["w_in"] = f(inp["w_in"][:NL])
    sh["gla_w_out"] = f(inp["gla_w_out"][:NL])
    sh["sgu_w_out"] = f(inp["sgu_w_out"][:NL])
    sh["conv_w_out"] = f(inp["conv_w_out"][:NL])
    sh["w_o"] = f(inp["w_o"][:NL])
    sh["ffn_w_up"] = f(inp["ffn_w_up"][:NL])
    sh["ffn_w_down"] = f(inp["ffn_w_down"][:NL])
    sh["walpha17"] = f(np.concatenate([np.asarray(inp["gla_w_alpha"])[:NL], np.asarray(inp["gla_b_alpha"])[:NL, None, :]], axis=1))
    sh["wsT"] = f(np.transpose(np.asarray(inp["sgu_ws"])[:NL], (0, 3, 1, 2)).reshape(NL, 128, 512))
    cv = np.zeros((128, NL * CV_L + 8), np.float32)
    pm = lambda v, n: np.asarray(v, dtype=np.float32).reshape(n, 128).T
    for l in range(NL):
        b = l * CV_L
        cv[:, b:b + 8] = pm(inp["norm_mix"][l], 8)
        cv[:, b + 8:b + 16] = pm(inp["norm_ffn"][l], 8)
        cv[:, b + 16:b + 20] = pm(inp["gla_norm"][l], 4)
        for kk in range(3):
            cv[:, b + 20 + kk * 4:b + 24 + kk * 4] = pm(inp["conv_w"][l][kk], 4)
            cv[:, b + 32 + kk * 44:b + 32 + (kk + 1) * 44] = pm(inp["ffn_conv_w"][l][kk], 44)
        cv[:, b + 164:b + 208] = pm(inp["ffn_conv_b"][l], 44)
    cv[:, NL * CV_L:NL * CV_L + 8] = pm(inp["norm_final"], 8)
    sh["cvec"] = cv
    rb = np.zeros((NL, 128, 1536), np.float32)
    for l in range(NL):
        rb[l, :, 0:512] = np.asarray(inp["sgu_ln_g"])[l][None, :]
        rb[l, :, 512:1024] = np.asarray(inp["sgu_ln_b"])[l][None, :]
        rb[l, :, 1024:1536] = np.asarray(inp["sgu_bs"])[l].reshape(1, 512)
    sh["rowb"] = rb
    j = np.arange(128)[:, None]
    i = np.arange(128)[None, :]
    cm = np.zeros((128, 768), np.float32)
    cm[:, 0:128] = np.where(j <= i, -1.0 / 16.0, 0.0)
    cm[:, 128:256] = np.where(j > i, -1.0 / 16.0, 0.0)
    cm[:, 256:768] = np.tile(np.where(j <= i, 1.0, 0.0), (1, 4))
    sh["cmat"] = cm
    return sh


_CACHE = {}


def run(inputs, T, NL, ncores):
    key = (T, NL)
    if key not in _CACHE:
        _CACHE[key] = build(T, NL)
    nc = _CACHE[key]
    shared = prep_inputs(inputs, NL)
    x = np.asarray(inputs["x"], dtype=np.float32)
    in_maps = []
    for c in range(ncores):
        m = dict(shared)
        m["xT"] = np.ascontiguousarray(x[c, :T].T)
        in_maps.append(m)
    res = run_bass_kernel_spmd(nc, in_maps, core_ids=list(range(ncores)))
    out = np.stack([np.ascontiguousarray(r["yT"].T) for r in res.results], axis=0)
    return out.astype(np.float32)


def kernel(**inputs):
    return run(inputs, 2048, 4, 8)
```
